# Optimizing a Trainium2 kernel written in Bass

```python
import jax, jax.numpy as jnp
from jax import lax
import numpy as np

D_MODEL = 1024
BATCH = 2
SEQ = 8192
DEPTH = 2

D_MIX = D_MODEL
D_GMLP = D_MIX // 2
D_CONV = D_MIX - D_GMLP
GMLP_HEADS = 4
GMLP_HEAD_DIM = D_GMLP // GMLP_HEADS
CHUNK = 128
CONV_WIDTH = 31
CONV_GROUPS = 4
D_IN = 2 * D_GMLP + 2 * D_CONV
N_EXPERTS = 16
N_EXPERT_GROUPS = 4
EXPERTS_PER_GROUP = N_EXPERTS // N_EXPERT_GROUPS
TOP_K = 2
D_EXPERT = D_MODEL // 2
DEEPNORM_ALPHA = (2.0 * DEPTH) ** 0.25
DEEPNORM_BETA = (8.0 * DEPTH) ** -0.25
LN_EPS = 1e-5

kernel_name = "hybrid_gmlp_conformer_grouped_moe_deepnorm"


def _normalize(x):
    xf = x.astype(jnp.float32)
    mu = jnp.mean(xf, axis=-1, keepdims=True)
    var = jnp.mean(jnp.square(xf - mu), axis=-1, keepdims=True)
    return (xf - mu) * lax.rsqrt(var + LN_EPS)


def layer_norm(x, g, b):
    y = _normalize(x) * g.astype(jnp.float32) + b.astype(jnp.float32)
    return y.astype(x.dtype)


def gmlp_spatial_gating(u, v, v_ln_g, v_ln_b, w_s, b_s):
    B, S, _ = v.shape
    n_chunks = S // CHUNK
    vh = _normalize(v.reshape(B, S, GMLP_HEADS, GMLP_HEAD_DIM))
    vh = vh * v_ln_g.reshape(GMLP_HEADS, GMLP_HEAD_DIM).astype(jnp.float32) \
        + v_ln_b.reshape(GMLP_HEADS, GMLP_HEAD_DIM).astype(jnp.float32)
    vh = vh.astype(v.dtype).reshape(B, n_chunks, CHUNK, GMLP_HEADS, GMLP_HEAD_DIM)
    causal = jnp.tril(jnp.ones((CHUNK, CHUNK), dtype=bool))
    w = jnp.where(causal[None], w_s, jnp.zeros_like(w_s)).astype(v.dtype)
    mixed = jnp.einsum('hij,bnjhd->bnihd', w, vh) \
        + b_s.T.astype(v.dtype)[None, None, :, :, None]
    return u * mixed.reshape(B, S, D_GMLP)


def conformer_conv(a, g, conv_w, conv_b, gn_g, gn_b):
    h = a * jax.nn.sigmoid(g)
    B, S, C = h.shape
    h = lax.conv_general_dilated(
        h, conv_w[:, None, :].astype(h.dtype), window_strides=(1,),
        padding=[(CONV_WIDTH - 1, 0)], dimension_numbers=('NWC', 'WIO', 'NWC'),
        feature_group_count=C) + conv_b.astype(h.dtype)
    hn = _normalize(h.reshape(B, S, CONV_GROUPS, C // CONV_GROUPS)).reshape(B, S, C)
    hn = (hn * gn_g.astype(jnp.float32) + gn_b.astype(jnp.float32)).astype(h.dtype)
    return jax.nn.silu(hn)


def hybrid_mixer(x, w_in, b_in, v_ln_g, v_ln_b, w_s, b_s, conv_w, conv_b, gn_g, gn_b,
                 w_out, b_out):
    z = jnp.einsum('bsd,df->bsf', x, w_in) + b_in
    u, v, a, g = jnp.split(z, [D_GMLP, 2 * D_GMLP, 2 * D_GMLP + D_CONV], axis=-1)
    y_a = gmlp_spatial_gating(jax.nn.gelu(u), jax.nn.gelu(v), v_ln_g, v_ln_b, w_s, b_s)
    y_b = conformer_conv(a, g, conv_w, conv_b, gn_g, gn_b)
    y = jnp.concatenate([y_a, y_b], axis=-1)
    return jnp.einsum('bsf,fd->bsd', y, w_out) + b_out


def grouped_moe(x, w_router, router_bias, w_gate, w_up, w_down):
    B, S, D = x.shape
    t = x.reshape(B * S, D)
    T = t.shape[0]
    logits = t.astype(jnp.float32) @ w_router.astype(jnp.float32)
    scores = jax.nn.softmax(logits, axis=-1)
    sel = (scores + router_bias.astype(jnp.float32)).reshape(T, N_EXPERT_GROUPS, EXPERTS_PER_GROUP)
    group_score = lax.top_k(sel, TOP_K)[0].sum(-1)
    group = jnp.argmax(group_score, axis=-1)
    in_group = jnp.take_along_axis(sel, group[:, None, None], axis=1)[:, 0]
    _, local = lax.top_k(in_group, TOP_K)
    expert = group[:, None] * EXPERTS_PER_GROUP + local
    gate = jnp.take_along_axis(scores, expert, axis=-1)
    gate = gate / jnp.sum(gate, axis=-1, keepdims=True)
    combine = jnp.einsum('tk,tke->te', gate,
                         jax.nn.one_hot(expert, N_EXPERTS, dtype=jnp.float32)).astype(x.dtype)
    h = jax.nn.silu(jnp.einsum('td,edf->tef', t, w_gate)) * jnp.einsum('td,edf->tef', t, w_up)
    h = h * combine[:, :, None]
    out = jnp.einsum('tef,efd->td', h, w_down)
    return out.reshape(B, S, D)


def setup_inputs(seed: int = 0) -> dict:
    key = jax.random.key(seed)
    ks = jax.random.split(key, 24)
    f32 = jnp.float32

    def nrm(k, shape, scale):
        return jax.random.normal(k, shape, f32) * scale

    return {
        "x": jax.random.normal(ks[0], (BATCH, SEQ, D_MODEL), f32),
        "w_in": nrm(ks[1], (DEPTH, D_MODEL, D_IN), D_MODEL ** -0.5),
        "b_in": nrm(ks[2], (DEPTH, D_IN), 0.02),
        "v_ln_g": 1.0 + nrm(ks[3], (DEPTH, D_GMLP), 0.05),
        "v_ln_b": nrm(ks[4], (DEPTH, D_GMLP), 0.02),
        "w_spatial": nrm(ks[5], (DEPTH, GMLP_HEADS, CHUNK, CHUNK), CHUNK ** -0.5),
        "b_spatial": 1.0 + nrm(ks[6], (DEPTH, GMLP_HEADS, CHUNK), 0.1),
        "conv_w": nrm(ks[7], (DEPTH, CONV_WIDTH, D_CONV), CONV_WIDTH ** -0.5),
        "conv_b": nrm(ks[8], (DEPTH, D_CONV), 0.02),
        "gn_g": 1.0 + nrm(ks[9], (DEPTH, D_CONV), 0.05),
        "gn_b": nrm(ks[10], (DEPTH, D_CONV), 0.02),
        "w_out": nrm(ks[11], (DEPTH, D_MIX, D_MODEL), DEEPNORM_BETA * D_MIX ** -0.5),
        "b_out": nrm(ks[12], (DEPTH, D_MODEL), 0.02),
        "ln1_g": 1.0 + nrm(ks[13], (DEPTH, D_MODEL), 0.05),
        "ln1_b": nrm(ks[14], (DEPTH, D_MODEL), 0.02),
        "w_router": nrm(ks[15], (D_MODEL, N_EXPERTS), D_MODEL ** -0.5),
        "router_bias": nrm(ks[16], (N_EXPERTS,), 0.01),
        "w_gate": nrm(ks[17], (DEPTH, N_EXPERTS, D_MODEL, D_EXPERT), D_MODEL ** -0.5),
        "w_up": nrm(ks[18], (DEPTH, N_EXPERTS, D_MODEL, D_EXPERT), D_MODEL ** -0.5),
        "w_down": nrm(ks[19], (DEPTH, N_EXPERTS, D_EXPERT, D_MODEL), DEEPNORM_BETA * D_EXPERT ** -0.5),
        "ln2_g": 1.0 + nrm(ks[20], (DEPTH, D_MODEL), 0.05),
        "ln2_b": nrm(ks[21], (DEPTH, D_MODEL), 0.02),
    }


def reference(x, w_in, b_in, v_ln_g, v_ln_b, w_spatial, b_spatial, conv_w, conv_b, gn_g, gn_b,
              w_out, b_out, ln1_g, ln1_b, w_router, router_bias, w_gate, w_up, w_down,
              ln2_g, ln2_b):
    for l in range(DEPTH):
        mix = hybrid_mixer(x, w_in[l], b_in[l], v_ln_g[l], v_ln_b[l], w_spatial[l], b_spatial[l],
                           conv_w[l], conv_b[l], gn_g[l], gn_b[l], w_out[l], b_out[l])
        x = layer_norm(DEEPNORM_ALPHA * x + mix, ln1_g[l], ln1_b[l])
        ffn = grouped_moe(x, w_router, router_bias, w_gate[l], w_up[l], w_down[l])
        x = layer_norm(DEEPNORM_ALPHA * x + ffn, ln2_g[l], ln2_b[l])
    return x
```

```python
import numpy as np
import concourse.bass as bass
import concourse.mybir as mybir
from concourse.bass_utils import run_bass_kernel_spmd

F32 = mybir.dt.float32
BF16 = mybir.dt.bfloat16
AF = mybir.ActivationFunctionType
ALU = mybir.AluOpType
AX = mybir.AxisListType

D = 1024
DEPTH = 2
NT = 17
TOK = NT * 128
NE = 16
ALPHA = (2.0 * DEPTH) ** 0.25
EPS = 1e-5
CW = 31
NCORES = 8

DT_SIZE = {F32: 4, BF16: 2}
CELL = 256


class Op:
    __slots__ = ("eng", "fn", "deps", "dma", "idx", "sig", "sigval", "sem", "semval")


class Sched:
    ENGS = ("pe", "act", "dve", "pool", "sp")
    NDMA = {"pool": 12, "sp": 8, "act": 4}

    def __init__(self):
        self.ops = {e: [] for e in self.ENGS}
        self.cells = {}
        self.dma_n = {q: 0 for q in self.NDMA}

    @staticmethod
    def _is_chip(ap):
        return type(ap.tensor).__name__ in ("SBTensorHandle", "PSumTensorHandle")

    def _cells(self, ap):
        t = ap.tensor
        sz = DT_SIZE[ap.dtype]
        shp = list(t.shape)
        row = 1
        for s in shp[1:]:
            row *= int(s)
        col = int(ap.offset) % row
        pat = [(int(s), int(c)) for (s, c) in ap.ap][1:]
        name = t.name
        if not pat:
            pat = [(1, 1)]
        if type(t).__name__ == "PSumTensorHandle":
            lo = col
            hi = col
            for (s, c) in pat:
                hi += s * (c - 1)
            return {(name, bnk) for bnk in range((lo * sz) // 2048, (hi * sz) // 2048 + 1)}
        inner_s, inner_c = pat[-1]
        outer = pat[:-1]
        nouter = 1
        for (_, c) in outer:
            nouter *= c
        res = set()
        if nouter > 256:
            lo = col
            hi = col + 1
            for (s, c) in pat:
                hi += s * (c - 1)
            for cc in range((lo * sz) // CELL, (hi * sz - 1) // CELL + 1):
                res.add((name, cc))
            return res
        offs = [col]
        for (s, c) in outer:
            offs = [o + s * i for o in offs for i in range(c)]
        span = inner_s * (inner_c - 1) + 1
        for o in offs:
            for cc in range((o * sz) // CELL, ((o + span) * sz - 1) // CELL + 1):
                res.add((name, cc))
        return res

    def add(self, eng, fn, reads=(), writes=(), dma=False, extra_deps=()):
        op = Op()
        op.eng, op.fn, op.dma, op.sig = eng, fn, dma, False
        op.sigval = op.sem = op.semval = None
        deps = {}
        for d in extra_deps:
            deps[id(d)] = d
        for ap in reads:
            if ap is None or isinstance(ap, (int, float)) or not self._is_chip(ap):
                continue
            for c in self._cells(ap):
                st = self.cells.get(c)
                if st is None:
                    st = self.cells[c] = [None, {}, []]
                if st[0] is not None:
                    deps[id(st[0])] = st[0]
                if dma:
                    st[2].append(op)
                else:
                    st[1][eng] = op
        for ap in writes:
            if ap is None or not self._is_chip(ap):
                continue
            for c in self._cells(ap):
                st = self.cells.get(c)
                if st is None:
                    st = self.cells[c] = [None, {}, []]
                if st[0] is not None:
                    deps[id(st[0])] = st[0]
                for r in st[1].values():
                    deps[id(r)] = r
                for r in st[2]:
                    deps[id(r)] = r
                st[0], st[1], st[2] = op, {}, []
        op.idx = len(self.ops[eng])
        best = {}
        final = []
        for d in deps.values():
            if d is op:
                continue
            if d.dma:
                final.append(d)
                continue
            if d.eng == "pe" and eng == "pe" and not dma:
                continue
            b = best.get(d.eng)
            if b is None or d.idx > b.idx:
                best[d.eng] = d
        for d in best.values():
            d.sig = True
            final.append(d)
        op.deps = final
        if dma:
            k = self.NDMA[eng]
            j = self.dma_n[eng]
            self.dma_n[eng] = j + 1
            op.sem = (eng, j % k)
            op.semval = 16 * (j // k + 1)
        self.ops[eng].append(op)
        return op

    def emit(self, nc, block, sems):
        for eng in self.ENGS:
            cnt = 0
            for op in self.ops[eng]:
                if op.sig and not op.dma:
                    cnt += 1
                    op.sigval = cnt

        def run(eng):
            lst = self.ops[eng]

            def body(e):
                waited = {}
                for op in lst:
                    for d in op.deps:
                        if d.dma:
                            key, val = ("dma",) + d.sem, d.semval
                        else:
                            key, val = ("eng", d.eng), d.sigval
                        if waited.get(key, 0) < val:
                            e.wait_ge(sems[key], val)
                            waited[key] = val
                    if op.dma:
                        key = ("dma",) + op.sem
                        prev = op.semval - 16
                        if prev > 0 and waited.get(key, 0) < prev:
                            e.wait_ge(sems[key], prev)
                            waited[key] = prev
                    if op.fn is None:
                        continue
                    ins = op.fn(e)
                    if op.dma:
                        ins.then_inc(sems[("dma",) + op.sem], 16)
                    elif op.sig:
                        ins.then_inc(sems[("eng", eng)], 1)
            return body

        block.tensor(run("pe"))
        block.scalar(run("act"))
        block.vector(run("dve"))
        block.gpsimd(run("pool"))
        block.sync(run("sp"))


class Arena:
    def __init__(self, ap_f32, words):
        self.base = ap_f32
        self.words = words
        self.off = 0

    def reset(self, off=0):
        self.off = off

    def alloc(self, shape, dtype):
        n = 1
        for s in shape[1:]:
            n *= s
        nbytes = n * DT_SIZE[dtype]
        nbytes = (nbytes + CELL - 1) // CELL * CELL
        w = nbytes // 4
        assert self.off + w <= self.words, ("arena overflow", self.off, w, self.words)
        v = self.base[0:shape[0], self.off:self.off + w]
        self.off += w
        if dtype != F32:
            v = v.bitcast(dtype)
        v = v[:, 0:n]
        if len(shape) == 2:
            return v
        names = "abcdefg"[: len(shape) - 1]
        pat = "p (" + " ".join(names) + ") -> p " + " ".join(names)
        kw = {names[i]: shape[i + 1] for i in range(len(names) - 1)}
        return v.rearrange(pat, **kw)


class _Stop(Exception):
    pass


def build_program(stop=None):
    nc = bass.Bass("TRN2", target_bir_lowering=False)
    dr = {}

    def din(name, shape):
        dr[name] = nc.dram_tensor(name, list(shape), F32, kind="ExternalInput").ap()
        return dr[name]

    x_d = din("x", (TOK, D))
    hm_d = din("hm", (128, 1))
    w_in_d = din("w_in", (DEPTH, D, 2048))
    b_in_d = din("b_in", (DEPTH, 2048))
    vg_d = din("v_ln_g", (DEPTH, 512))
    vb_d = din("v_ln_b", (DEPTH, 512))
    wsp_d = din("w_spatial", (DEPTH, 4, 128, 128))
    bsp_d = din("b_spatial", (DEPTH, 4, 128))
    cw_d = din("conv_w", (DEPTH, CW, 512))
    cb_d = din("conv_b", (DEPTH, 512))
    gg_d = din("gn_g", (DEPTH, 512))
    gb_d = din("gn_b", (DEPTH, 512))
    w_out_d = din("w_out", (DEPTH, D, D))
    b_out_d = din("b_out", (DEPTH, D))
    l1g_d = din("ln1_g", (DEPTH, D))
    l1b_d = din("ln1_b", (DEPTH, D))
    wr_d = din("w_router", (D, NE))
    rb_d = din("router_bias", (NE,))
    wg_d = din("w_gate", (DEPTH, NE, D, 512))
    wu_d = din("w_up", (DEPTH, NE, D, 512))
    wd_d = din("w_down", (DEPTH, NE, 512, D))
    l2g_d = din("ln2_g", (DEPTH, D))
    l2b_d = din("ln2_b", (DEPTH, D))
    out_d = nc.dram_tensor("out", [2048, D], F32, kind="ExternalOutput").ap()

    S = Sched()
    ckpt = [0]

    def chk(name):
        ckpt[0] += 1
        if stop is not None and ckpt[0] == stop:
            print('STOP at', name)
            raise _Stop()
    AW = 101 * 256 - 64

    from contextlib import ExitStack
    with ExitStack() as es:
        def sb(name, shape, dt):
            return es.enter_context(nc.sbuf_tensor(name, list(shape), dt))

        xs = sb("xs", (128, NT, D), F32)
        xT = sb("xT", (128, 8, TOK), BF16)
        ident = sb("ident", (128, 128), F32)
        cmat = sb("cmat", (128, 128), F32)
        odiv = sb("odiv", (128, 128), BF16)
        ones_row = sb("ones_row", (1, 128), F32)
        wr = sb("wr", (128, 8, NE), F32)
        rb_bc = sb("rb_bc", (128, NE), F32)
        hm = sb("hm_sb", (128, 1), F32)
        lg = sb("lg", (128, NT, NE), F32)
        comb = sb("comb", (128, NT, NE), F32)
        lnst_a = sb("lnst", (128, 4, 12), F32)
        lnmv_a = sb("lnmv", (128, 4, 2), F32)
        lnr_a = sb("lnr", (128, 4, 2), F32)
        epst = sb("epst", (128, 1), F32)
        arena_t = sb("arena", (128, AW), F32)
        ps = es.enter_context(nc.psum_tensor("ps", [128, 8, 512], F32))
        A = Arena(arena_t[:, :], AW)

        sems = {}
        for eng in Sched.ENGS:
            sems[("eng", eng)] = es.enter_context(nc.semaphore("s_" + eng))
        for q, k in Sched.NDMA.items():
            for i in range(k):
                sems[("dma", q, i)] = es.enter_context(nc.semaphore("d_%s%d" % (q, i)))

        def mm(out, lhsT, rhs, start, stop, tp=None):
            if tp is None:
                S.add("pe", lambda e: e.matmul(out, lhsT, rhs, start=start, stop=stop),
                      reads=[lhsT, rhs], writes=[out])
            else:
                S.add("pe", lambda e: e.matmul(out, lhsT, rhs, start=start, stop=stop, tile_position=tp),
                      reads=[lhsT, rhs], writes=[out])

        def tr(out, in_, idn):
            S.add("pe", lambda e: e.transpose(out, in_, idn), reads=[in_, idn], writes=[out])

        def act(out, in_, func, bias=None, scale=None, eng="act"):
            kw = {}
            if bias is not None:
                kw["bias"] = bias
            if scale is not None:
                kw["scale"] = scale
            S.add(eng, lambda e: e.activation(out=out, in_=in_, func=func, **kw),
                  reads=[in_, bias, scale], writes=[out])

        def tsc(eng, out, in0, s1, s2, op0, op1=None):
            if op1 is None:
                S.add(eng, lambda e: e.tensor_scalar(out=out, in0=in0, scalar1=s1, scalar2=None, op0=op0),
                      reads=[in0, s1], writes=[out])
            else:
                S.add(eng, lambda e: e.tensor_scalar(out=out, in0=in0, scalar1=s1, scalar2=s2, op0=op0, op1=op1),
                      reads=[in0, s1, s2], writes=[out])

        def stt(eng, out, in0, scalar, in1, op0, op1):
            S.add(eng, lambda e: e.scalar_tensor_tensor(out=out, in0=in0, scalar=scalar, in1=in1, op0=op0, op1=op1),
                  reads=[in0, scalar, in1], writes=[out])

        def tt(eng, out, in0, in1, op):
            S.add(eng, lambda e: e.tensor_tensor(out=out, in0=in0, in1=in1, op=op),
                  reads=[in0, in1], writes=[out])

        def cp(eng, out, in_):
            S.add(eng, lambda e: e.tensor_copy(out=out, in_=in_), reads=[in_], writes=[out])

        def red(eng, out, in_, op):
            S.add(eng, lambda e: e.tensor_reduce(out=out, in_=in_, axis=AX.X, op=op), reads=[in_], writes=[out])

        def dma(q, out, in_):
            return S.add(q, lambda e: e.dma_start(out=out, in_=in_), reads=[in_], writes=[out], dma=True)

        def memset(eng, ap, val):
            S.add(eng, lambda e: e.memset(ap, val), writes=[ap])

        def bank(i, n=512):
            return ps[:, i, 0:n]

        memset("pool", ident[:, :], 1.0)
        S.add("pool", lambda e: e.affine_select(out=ident[:, :], in_=ident[:, :], pattern=[[-1, 128]],
                                                compare_op=ALU.is_ge, fill=0.0, base=0, channel_multiplier=1),
              reads=[ident[:, :]], writes=[ident[:, :]])
        S.add("pool", lambda e: e.affine_select(out=ident[:, :], in_=ident[:, :], pattern=[[1, 128]],
                                                compare_op=ALU.is_ge, fill=0.0, base=0, channel_multiplier=-1),
              reads=[ident[:, :]], writes=[ident[:, :]])
        memset("pool", odiv[:, :], 1.0 / 128.0)
        memset("pool", ones_row[:, :], 1.0)
        memset("pool", epst[:, :], EPS)
        tsc("dve", cmat[:, :], ident[:, :], 1.0 / 128.0, None, ALU.subtract)

        dma("sp", wr[:, :, :], wr_d.rearrange("(k p) e -> p k e", p=128))
        dma("sp", rb_bc[:, :], rb_d.partition_broadcast(128))
        dma("sp", hm[:, :], hm_d)
        xv = x_d.rearrange("(i p) d -> p i d", p=128)
        for (i0, i1) in ((0, 3), (3, 5), (5, 9), (9, 13), (13, 17)):
            dma("sp", xs[:, i0:i1, :], xv[:, i0:i1, :])

        def build_xT(i, tr_bank, lg_ps, xT32, router):
            for h in range(2):
                pb = ps[:, tr_bank, :].rearrange("p (a b) -> p a b", a=4)
                for j in range(4):
                    tr(pb[:, j, :], xs[:, i, (4 * h + j) * 128:(4 * h + j + 1) * 128], ident[:, :])
                yield
                cp("dve", xT[:, 4 * h:4 * h + 4, i * 128:(i + 1) * 128], pb)
                if router:
                    cp("dve", xT32[:, 4 * h:4 * h + 4, :], pb)
                yield
            if router:
                for k in range(8):
                    mm(lg_ps, xT32[:, k, :], wr[:, k, :], k == 0, k == 7)
                yield
                cp("dve", lg[:, i, :], lg_ps)
                yield

        def layer_norm_tile(i, g_bc, b_bc, q=0):
            xt = xs[:, i, :]
            lnst, lnmv, lnr = lnst_a[:, q, :], lnmv_a[:, q, :], lnr_a[:, q, :]
            for c in range(2):
                S.add("dve", lambda e, o=lnst[:, 6 * c:6 * c + 6], s=xs[:, i, c * 512:(c + 1) * 512]: e.bn_stats(out=o, in_=s),
                      reads=[xs[:, i, c * 512:(c + 1) * 512]], writes=[lnst[:, 6 * c:6 * c + 6]])
            yield
            S.add("dve", lambda e: e.bn_aggr(out=lnmv[:, :], in_=lnst[:, :]), reads=[lnst[:, :]], writes=[lnmv[:, :]])
            yield
            act(lnr[:, 0:1], lnmv[:, 1:2], AF.Sqrt, bias=epst[:, 0:1])
            yield
            S.add("dve", lambda e: e.reciprocal(out=lnr[:, 0:1], in_=lnr[:, 0:1]), reads=[lnr[:, 0:1]], writes=[lnr[:, 0:1]])
            tsc("dve", lnr[:, 1:2], lnmv[:, 0:1], lnr[:, 0:1], -1.0, ALU.mult, ALU.mult)
            yield
            act(xt, xt, AF.Identity, bias=lnr[:, 1:2], scale=lnr[:, 0:1])
            yield
            tt("dve", xt, xt, g_bc, ALU.mult)
            yield
            tt("dve", xt, xt, b_bc, ALU.add)
            yield

        def run_threads(ths, weights=None):
            ths = [t for t in ths if t is not None]
            wts_ = dict(zip(map(id, ths), weights or [1] * len(ths)))
            while ths:
                for t in list(ths):
                    for _ in range(wts_.get(id(t), 1)):
                        try:
                            next(t)
                        except StopIteration:
                            ths.remove(t)
                            break

        def drain(gen):
            for _ in gen:
                pass

        for i in range(NT):
            drain(build_xT(i, 6 + (i % 2), None, None, False))

        out_dmas = []
        try:
          for l in range(DEPTH):
            last = (l == DEPTH - 1)
            A.reset()
            wi = [A.alloc((128, 8, 512), BF16) for _ in range(4)]
            wo = [A.alloc((128, 8, 512), BF16) for _ in range(2)]
            pcol64 = A.alloc((128, 64), F32)
            pcol = pcol64[:, 0:32]
            cbc = pcol64[:, 32:36]
            cwc = A.alloc((128, 124), F32)
            WmT = A.alloc((128, 4, 128), BF16)
            Ch = A.alloc((128, 4, 128), F32)
            bv_bc = A.alloc((128, 512), F32)
            bout_bc = A.alloc((128, D), F32)
            l1g_bc = A.alloc((128, D), F32)
            l1b_bc = A.alloc((128, D), F32)
            xT32 = A.alloc((128, 8, 128), F32)
            NB = 256
            scratch0 = A.off
            guT = A.alloc((128, 4, NB), BF16)
            sig = [A.alloc((128, NB), F32) for _ in range(1)]
            hbufs = [A.alloc((128, 4, 30 + NB), BF16) for _ in range(2)]
            DkH = [A.alloc((128, 16, 128), BF16) for _ in range(2)]
            cacc = [A.alloc((128, NB), F32) for _ in range(2)]
            sqs = [A.alloc((128, NB), BF16) for _ in range(2)]
            yT = [A.alloc((128, 8, NB), BF16) for _ in range(2)]
            t1 = [A.alloc((128, 128), F32)] * 2
            vsm = A.alloc((128, 64), F32)
            vst = vsm[:, 0:24].rearrange('p (a b) -> p a b', a=4)
            vmv = vsm[:, 24:32].rearrange('p (a b) -> p a b', a=4)
            vr = vsm[:, 32:36]
            zv = A.alloc((128, 512), F32)
            gv = zv
            nb = A.alloc((128, 512), BF16)
            A.reset(scratch0)
            stA = A.alloc((128, 128), F32)
            stB = A.alloc((128, 128), F32)
            wsp = A.alloc((128, 4, 128), F32)
            WmT32 = A.alloc((128, 4, 128), F32)
            bcb = A.alloc((128, 512), F32)
            bs_row = A.alloc((1, 4, 128), F32)

            for j in (2, 3, 0, 1):
                dma("pool", wi[j][:, :, :], w_in_d[l, :, j * 512:(j + 1) * 512].rearrange("(k p) f -> p k f", p=128))
            for j in range(2):
                dma("pool", wo[j][:, :, :], w_out_d[l, :, j * 512:(j + 1) * 512].rearrange("(k p) f -> p k f", p=128))
            dma("sp", stA[0:16, :], b_in_d[l].rearrange("(r p) -> r p", p=128))
            dma("sp", stA[16:20, :], cb_d[l].rearrange("(r p) -> r p", p=128))
            dma("sp", stA[20:24, :], gg_d[l].rearrange("(r p) -> r p", p=128))
            dma("sp", stA[24:28, :], gb_d[l].rearrange("(r p) -> r p", p=128))
            dma("sp", stA[28:32, :], vg_d[l].rearrange("(r p) -> r p", p=128))
            dma("sp", stB[0:124, :], cw_d[l].rearrange("k (c p) -> (k c) p", p=128))
            dma("sp", wsp[:, :, :], wsp_d[l].rearrange("h i j -> i h j"))
            dma("sp", bcb[:, :], vb_d[l].partition_broadcast(128))
            dma("sp", bs_row[:, :, :], bsp_d[l].rearrange("(o h) j -> o h j", o=1))
            dma("sp", bv_bc[:, :], b_in_d[l, 512:1024].partition_broadcast(128))
            dma("sp", bout_bc[:, :], b_out_d[l].partition_broadcast(128))
            dma("sp", l1g_bc[:, :], l1g_d[l].partition_broadcast(128))
            dma("sp", l1b_bc[:, :], l1b_d[l].partition_broadcast(128))

            tr(ps[:, 3, 0:32], stA[0:32, :], ident[0:32, 0:32])
            cp("dve", pcol[:, :], ps[:, 3, 0:32])
            tr(ps[:, 4, 0:124], stB[0:124, :], ident[0:124, 0:124])
            cp("dve", cwc[:, :], ps[:, 4, 0:124])
            mm(ps[:, 4, 0:4], cmat[:, :], pcol[:, 16:20], True, True)
            cp("dve", cbc[:, :], ps[:, 4, 0:4])
            S.add("pool", lambda e, w=wsp: e.affine_select(out=w[:, :, :], in_=w[:, :, :], pattern=[[0, 4], [-1, 128]],
                                                            compare_op=ALU.is_ge, fill=0.0, base=0, channel_multiplier=1),
                  reads=[wsp[:, :, :]], writes=[wsp[:, :, :]])
            pb3 = ps[:, 3, :].rearrange("p (a b) -> p a b", a=4)
            for h in range(4):
                tr(pb3[:, h, :], wsp[:, h, :], ident[:, :])
            cp("dve", WmT[:, :, :], pb3)
            cp("dve", WmT32[:, :, :], pb3)
            pb4 = ps[:, 4, :].rearrange("p (a b) -> p a b", a=4)
            for h in range(4):
                mm(pb4[:, h, :], bcb[:, h * 128:(h + 1) * 128], WmT32[:, h, :], True, False)
                mm(pb4[:, h, :], ones_row[0:1, :], bs_row[0:1, h, :], False, True)
            cp("dve", Ch[:, :, :], pb4)

            chk('L%d setup' % l)
            if l == 0:
                blocks = [(0, 128)] + [(128 + NB * b, NB) for b in range(2048 // NB)]
            else:
                blocks = [(0, 128)] + [(128 + NB * b, NB) for b in range(2048 // NB)]
            slot_ctr = [0]

            def pslot(n):
                s = slot_ctr[0] % 2
                slot_ctr[0] += 1
                return ps[:, s, 0:n]

            lg_ps = ps[:, 4, 0:NE]

            nblk = len(blocks)
            F = {"h": [[False] * 4 for _ in range(nblk)], "conv": [[False] * 4 for _ in range(nblk)],
                 "p": [False] * nblk, "a": [False] * nblk, "v": [False] * nblk, "bo": [False] * nblk}

            def is_full(bi):
                return not (blocks[bi][0] == 0 and l > 0)

            def thread_p():
                for bi, (c0, n) in enumerate(blocks):
                    halo = (c0 == 0)
                    hbuf = hbufs[bi % 2]
                    if bi >= 2 and is_full(bi - 2):
                        while not all(F["conv"][bi - 2]):
                            yield
                    if halo:
                        memset("dve", hbuf[:, :, 0:30], 0.0)
                    else:
                        npv = blocks[bi - 1][1]
                        cp("dve", hbuf[:, :, 0:30], hbufs[(bi - 1) % 2][:, :, npv:npv + 30])
                    yield
                    for cc in range(4):
                        pg = ps[:, 0, 0:n]
                        for k in range(8):
                            mm(pg, wi[3][:, k, cc * 128:(cc + 1) * 128], xT[:, k, c0:c0 + n], k == 0, k == 7)
                        yield
                        sg_ = sig[0][:, 0:n]
                        act(sg_, pg, AF.Sigmoid, bias=pcol[:, 12 + cc:13 + cc])
                        yield
                        pa = ps[:, 0, 0:n]
                        for k in range(8):
                            mm(pa, wi[2][:, k, cc * 128:(cc + 1) * 128], xT[:, k, c0:c0 + n], k == 0, k == 7)
                        yield
                        hh = hbuf[:, cc, 30:30 + n]
                        stt("dve", hh, pa, pcol[:, 8 + cc:9 + cc], sg_, ALU.add, ALU.mult)
                        if halo:
                            tsc("dve", hh, hh, hm[:, 0:1], None, ALU.mult)
                        F["h"][bi][cc] = True
                        yield
                    F["p"][bi] = True

            F["acnt"] = [0] * nblk

            def thread_a(tid, ccs, bank_c, bank_v):
                cw3 = cwc[:, :].rearrange("p (k c) -> p k c", c=4)
                Dh = DkH[tid]
                sq = sqs[tid]
                for bi, (c0, n) in enumerate(blocks):
                    if not is_full(bi):
                        F["acnt"][bi] += 1
                        continue
                    hbuf = hbufs[bi % 2]
                    y = yT[bi % 2]
                    while bi >= 2 and is_full(bi - 2) and not F["bo"][bi - 2]:
                        yield
                    for cc in ccs:
                        pc = ps[:, bank_c, 0:n]
                        for (k0, k1) in ((0, 16), (16, CW)):
                            nk = k1 - k0
                            tt("dve", Dh[:, 0:nk, :], cmat[:, :].unsqueeze(1).to_broadcast([128, nk, 128]),
                               cw3[:, k0:k1, cc].unsqueeze(2).to_broadcast([128, nk, 128]), ALU.mult)
                            yield
                            while not F["h"][bi][cc]:
                                yield
                            for k in range(k0, k1):
                                mm(pc, Dh[:, k - k0, :], hbuf[:, cc, k:k + n], k == 0, k == CW - 1)
                                if k % 8 == 7:
                                    yield
                            yield
                        F["conv"][bi][cc] = True
                        vps = ps[:, bank_v, 0:n]
                        act(sq[:, 0:n], pc, AF.Square, bias=cbc[:, cc:cc + 1])
                        yield
                        mm(vps, odiv[:, :], sq[:, 0:n], True, True)
                        yield
                        acc = cacc[tid][:, 0:n]
                        act(acc, vps, AF.Sqrt, bias=epst[:, 0:1])
                        yield
                        S.add("dve", lambda e, o=acc: e.reciprocal(out=o, in_=o), reads=[acc], writes=[acc])
                        yield
                        stt("dve", acc, pc, cbc[:, cc:cc + 1], acc, ALU.add, ALU.mult)
                        yield
                        act(y[:, 4 + cc, 0:n], acc, AF.Silu, bias=pcol[:, 24 + cc:25 + cc], scale=pcol[:, 20 + cc:21 + cc])
                        yield
                    F["acnt"][bi] += 1

            def thread_v_blk(bi, c0, n):
                y = yT[bi % 2]
                for fc in range(4):
                    p = ps[:, 2, 0:n]
                    for k in range(8):
                        mm(p, wi[0][:, k, fc * 128:(fc + 1) * 128], xT[:, k, c0:c0 + n], k == 0, k == 7)
                    yield
                    act(guT[:, fc, 0:n], p, AF.Gelu_apprx_tanh, bias=pcol[:, fc:fc + 1])
                    yield
                for j in range(n // 128):
                    tc0 = c0 + j * 128
                    pv = bank(2)
                    for k in range(8):
                        mm(pv, xT[:, k, tc0:tc0 + 128], wi[1][:, k, :], k == 0, k == 7)
                    yield
                    tt("dve", zv[:, :], pv, bv_bc[:, :], ALU.add)
                    yield
                    act(gv[:, :], zv[:, :], AF.Gelu_apprx_tanh)
                    yield
                    for h in range(4):
                        S.add("dve", lambda e, o=vst[:, h, :], s=gv[:, h * 128:(h + 1) * 128]: e.bn_stats(out=o, in_=s),
                              reads=[gv[:, h * 128:(h + 1) * 128]], writes=[vst[:, h, :]])
                    yield
                    for h in range(4):
                        S.add("dve", lambda e, o=vmv[:, h, :], s=vst[:, h, :]: e.bn_aggr(out=o, in_=s),
                              reads=[vst[:, h, :]], writes=[vmv[:, h, :]])
                    yield
                    act(vr[:, :], vmv[:, :, 1], AF.Sqrt, bias=epst[:, 0:1])
                    yield
                    S.add("dve", lambda e: e.reciprocal(out=vr[:, :], in_=vr[:, :]), reads=[vr[:, :]], writes=[vr[:, :]])
                    yield
                    for h in range(4):
                        tsc("dve", nb[:, h * 128:(h + 1) * 128], gv[:, h * 128:(h + 1) * 128],
                            vmv[:, h, 0:1], vr[:, h:h + 1], ALU.subtract, ALU.mult)
                    yield
                    pm = ps[:, 2, :].rearrange("p (a b) -> p a b", a=4)
                    for h in range(4):
                        mm(pm[:, h, :], nb[:, h * 128:(h + 1) * 128], WmT[:, h, :], True, True)
                    yield
                    for h in range(4):
                        tt_ = t1[h % 2]
                        stt("dve", tt_[:, :], pm[:, h, :], pcol[:, 28 + h:29 + h], Ch[:, h, :], ALU.mult, ALU.add)
                        tt("dve", y[:, h, j * 128:(j + 1) * 128], tt_[:, :], guT[:, h, j * 128:(j + 1) * 128], ALU.mult)
                        yield

            def thread_v():
                for bi, (c0, n) in enumerate(blocks):
                    if is_full(bi):
                        while bi >= 2 and is_full(bi - 2) and not F["bo"][bi - 2]:
                            yield
                        for _ in thread_v_blk(bi, c0, n):
                            yield
                    F["v"][bi] = True

            def thread_b_blk(bi, c0, n):
                y = yT[bi % 2]
                for j in range(n // 128):
                    i = (c0 + j * 128) // 128
                    xt = xs[:, i, :]
                    for hf in range(2):
                        for k in range(8):
                            mm(ps[:, 4, :], y[:, k, j * 128:(j + 1) * 128], wo[hf][:, k, :], k == 0, k == 7)
                        if j == n // 128 - 1 and hf == 1:
                            F["bo"][bi] = True
                        yield
                        xh = xs[:, i, hf * 512:(hf + 1) * 512]
                        stt("dve", xh, xh, ALPHA, ps[:, 4, :], ALU.mult, ALU.add)
                        yield
                    tt("dve", xt, xt, bout_bc[:, :], ALU.add)
                    yield
                    for _ in layer_norm_tile(i, l1g_bc[:, :], l1b_bc[:, :]):
                        yield
                    for _ in build_xT(i, 5, lg_ps, xT32, True):
                        yield
                    act(xt, xt, AF.Identity, scale=ALPHA)
                    yield

            def thread_b():
                for bi, (c0, n) in enumerate(blocks):
                    if not is_full(bi):
                        F["bo"][bi] = True
                        continue
                    while not (F["p"][bi] and F["acnt"][bi] == 2 and F["v"][bi]):
                        yield
                    for _ in thread_b_blk(bi, c0, n):
                        yield

            run_threads([thread_p(), thread_a(0, (0, 2), 1, 7), thread_a(1, (1, 3), 3, 6), thread_v(), thread_b()],
                        weights=[2, 2, 2, 2, 1])
            chk('L%d mixer done' % l)

            A.reset()
            NU = 8
            ring = [A.alloc((128, 8, 512), BF16) for _ in range(NU)]
            hT = [A.alloc((128, 4, 512), BF16) for _ in range(2)]
            sgb = [A.alloc((128, 512), F32) for _ in range(2)]
            l2g_bc = A.alloc((128, D), F32)
            l2b_bc = A.alloc((128, D), F32)
            R = NT * NE
            rt = [A.alloc((128, NT, NE), F32) for _ in range(6)]
            rs1 = [A.alloc((128, NT * 4), F32) for _ in range(4)]
            rs2 = [A.alloc((128, NT), F32) for _ in range(4)]

            dma("sp", l2g_bc[:, :], l2g_d[l].partition_broadcast(128))
            dma("sp", l2b_bc[:, :], l2b_d[l].partition_broadcast(128))

            tiles0 = 0 if l == 0 else 1
            ntl = NT - tiles0
            lgv = lg[:, tiles0:NT, :]

            def bc3(a2, k):
                return a2.unsqueeze(2).to_broadcast([128, a2.shape[1], k])
            ex, sc, sel, e1, sel2, e2 = [r[:, tiles0:NT, :] for r in rt]
            mx, sm, rsm, den = [r[:, tiles0:NT] for r in rs2]
            m1, m2, gsc, geq = [r[:, tiles0 * 4:NT * 4] for r in rs1]
            red("dve", mx, lgv, ALU.max)
            tt("dve", ex, lgv, bc3(mx, NE), ALU.subtract)
            act(ex, ex, AF.Exp)
            red("dve", sm, ex, ALU.add)
            S.add("dve", lambda e, o=rsm, s=sm: e.reciprocal(out=o, in_=s), reads=[sm], writes=[rsm])
            tt("dve", sc, ex, bc3(rsm, NE), ALU.mult)
            tt("dve", sel, sc, rb_bc[:, :].unsqueeze(1).to_broadcast([128, ntl, NE]), ALU.add)
            sel_g = sel.rearrange("p t (g e) -> p (t g) e", e=4)
            e1_g = e1.rearrange("p t (g e) -> p (t g) e", e=4)
            sel2_g = sel2.rearrange("p t (g e) -> p (t g) e", e=4)
            e2_g = e2.rearrange("p t (g e) -> p (t g) e", e=4)
            red("dve", m1, sel_g, ALU.max)
            tt("dve", e1_g, sel_g, bc3(m1, 4), ALU.is_equal)
            stt("dve", sel2_g, e1_g, -1.0e30, sel_g, ALU.mult, ALU.add)
            red("dve", m2, sel2_g, ALU.max)
            tt("dve", e2_g, sel2_g, bc3(m2, 4), ALU.is_equal)
            tt("dve", gsc, m1, m2, ALU.add)
            gsc3 = gsc.rearrange("p (t g) -> p t g", g=4)
            red("dve", den, gsc3, ALU.max)
            geq3 = geq.rearrange("p (t g) -> p t g", g=4)
            tt("dve", geq3, gsc3, bc3(den, 4), ALU.is_equal)
            tt("dve", e1_g, e1_g, e2_g, ALU.add)
            tt("dve", e1_g, e1_g, bc3(geq, 4), ALU.mult)
            tt("dve", sel, e1, sc, ALU.mult)
            red("dve", sm, sel, ALU.add)
            S.add("dve", lambda e, o=rsm, s=sm: e.reciprocal(out=o, in_=s), reads=[sm], writes=[rsm])
            tt("dve", comb[:, tiles0:NT, :], sel, bc3(rsm, NE), ALU.mult)

            chk('L%d routing' % l)
            if l == 0:
                mblocks = [(0, 512), (512, 512), (1024, 384), (1408, 384), (1792, 384)]
            else:
                mblocks = [(128 + 512 * b, 512) for b in range(4)]
            unit_ctr = [0]

            def load_expert(e_):
                us = []
                for (src, pat) in ((wg_d[l, e_], "(k p) f -> p k f"), (wu_d[l, e_], "(k p) f -> p k f")):
                    u = ring[unit_ctr[0] % NU]
                    unit_ctr[0] += 1
                    dma("pool", u[:, :, :], src.rearrange(pat, p=128))
                    us.append(u)
                u = ring[unit_ctr[0] % NU]
                unit_ctr[0] += 1
                ud = u.rearrange("p k f -> p (k f)").rearrange("p (k f) -> p k f", k=4)
                dma("pool", ud, wd_d[l, e_].rearrange("(k p) f -> p k f", p=128))
                us.append(ud)
                return us

            wts = {0: load_expert(0), 1: load_expert(1)}
            work = [(e_, c0, n) for e_ in range(NE) for (c0, n) in mblocks]
            gslot = [0]

            def s1(wi_, idx):
                e_, c0, n = wi_
                Wg, Wu, _ = wts[e_]
                h_ = hT[idx % 2]
                for fc in range(4):
                    s = gslot[0] % 2
                    gslot[0] += 1
                    pg = ps[:, s, 0:n]
                    pu = ps[:, 2 + s, 0:n]
                    for k in range(8):
                        mm(pg, Wg[:, k, fc * 128:(fc + 1) * 128], xT[:, k, c0:c0 + n], k == 0, k == 7)
                    for k in range(8):
                        mm(pu, Wu[:, k, fc * 128:(fc + 1) * 128], xT[:, k, c0:c0 + n], k == 0, k == 7)
                    act(sgb[s][:, 0:n], pg, AF.Silu)
                    tt("dve", h_[:, fc, 0:n], sgb[s][:, 0:n], pu, ALU.mult)

            ytog = [0]

            def s2(wi_, idx):
                e_, c0, n = wi_
                Wd = wts[e_][2]
                h_ = hT[idx % 2]
                for j in range(n // 128):
                    i = (c0 + j * 128) // 128
                    yb = 4 + 2 * (ytog[0] % 2)
                    ytog[0] += 1
                    for hf in range(2):
                        for fc in range(4):
                            mm(ps[:, yb + hf, :], h_[:, fc, j * 128:(j + 1) * 128], Wd[:, fc, hf * 512:(hf + 1) * 512],
                               fc == 0, fc == 3)
                    xt2 = xs[:, i, :].rearrange("p (a b) -> p a b", a=2)
                    stt("dve", xt2, ps[:, yb:yb + 2, :], comb[:, i, e_:e_ + 1], xt2, ALU.mult, ALU.add)
                if (c0, n) == mblocks[-1] and e_ + 2 < NE:
                    wts[e_ + 2] = load_expert(e_ + 2)
                if e_ == NE - 1:
                    def ln2_thread(i, q):
                        for _ in layer_norm_tile(i, l2g_bc[:, :], l2b_bc[:, :], q):
                            yield
                        if not last:
                            for _ in build_xT(i, q, None, None, False):
                                yield
                    tl = [(c0 + j * 128) // 128 for j in range(n // 128)]
                    run_threads([ln2_thread(i, q) for q, i in enumerate(tl)])
                    if last:
                        ov = out_d.rearrange("(i p) d -> p i d", p=128)
                        if tl[0] >= 1:
                            out_dmas.append(dma("sp", ov[:, tl[0] - 1:tl[-1], :], xs[:, tl[0]:tl[-1] + 1, :]))

            for idx, w_ in enumerate(work):
                s1(w_, idx)
                if idx > 0:
                    s2(work[idx - 1], idx - 1)
            s2(work[-1], len(work) - 1)
            chk('L%d experts' % l)

            chk('L%d ln2' % l)
        except _Stop:
            ov = out_d.rearrange("(i p) d -> p i d", p=128)
            for (i0, i1) in ((1, 5), (5, 9), (9, 13), (13, 17)):
                out_dmas.append(dma("sp", ov[:, i0 - 1:i1 - 1, :], xs[:, i0:i1, :]))
        tail_deps = list(out_dmas)
        for _eng in Sched.ENGS:
            _real = [o for o in S.ops[_eng] if o.fn is not None]
            if _real:
                tail_deps.append(_real[-1])
            tail_deps.extend(o for o in S.ops[_eng] if o.dma)
        S.add("sp", None, extra_deps=tail_deps)

        with nc.Block() as block:
            S.emit(nc, block, sems)
    return nc


_NC = None


def kernel(**inputs):
    global _NC
    if _NC is None:
        _NC = build_program()
    x = np.ascontiguousarray(np.asarray(inputs["x"], dtype=np.float32))
    shared = {k: np.ascontiguousarray(np.asarray(v, dtype=np.float32)) for k, v in inputs.items() if k != "x"}
    in_maps = []
    for c in range(NCORES):
        b, q = c // 4, c % 4
        xc = np.zeros((TOK, D), np.float32)
        xc[128:] = x[b, q * 2048:(q + 1) * 2048]
        if q > 0:
            xc[:128] = x[b, q * 2048 - 128:q * 2048]
        m = dict(shared)
        m["x"] = xc
        m["hm"] = np.full((128, 1), 1.0 if q > 0 else 0.0, np.float32)
        in_maps.append(m)
    res = run_bass_kernel_spmd(_NC, in_maps, core_ids=list(range(NCORES)))
    out = np.empty((2, 8192, D), np.float32)
    for c in range(NCORES):
        b, q = c // 4, c % 4
        out[b, q * 2048:(q + 1) * 2048] = res.results[c]["out"]
    return out
```

```python
import numpy as np
import concourse.bass as bass
import concourse.mybir as mybir
from concourse.bass_utils import run_bass_kernel_spmd

F32 = mybir.dt.float32
BF16 = mybir.dt.bfloat16
AF = mybir.ActivationFunctionType
ALU = mybir.AluOpType
AX = mybir.AxisListType

D = 1024
DEPTH = 2
NT = 17
TOK = NT * 128
NE = 16
ALPHA = (2.0 * DEPTH) ** 0.25
EPS = 1e-5
CW = 31
NCORES = 8

DT_SIZE = {F32: 4, BF16: 2}
CELL = 256


class Op:
    __slots__ = ("eng", "fn", "deps", "dma", "idx", "sig", "sigval", "sem", "semval")


class Sched:
    ENGS = ("pe", "act", "dve", "pool", "sp")
    NDMA = {"pool": 12, "sp": 8, "act": 4}

    def __init__(self):
        self.ops = {e: [] for e in self.ENGS}
        self.cells = {}
        self.dma_n = {q: 0 for q in self.NDMA}

    @staticmethod
    def _is_chip(ap):
        return type(ap.tensor).__name__ in ("SBTensorHandle", "PSumTensorHandle")

    def _cells(self, ap):
        t = ap.tensor
        sz = DT_SIZE[ap.dtype]
        shp = list(t.shape)
        row = 1
        for s in shp[1:]:
            row *= int(s)
        col = int(ap.offset) % row
        pat = [(int(s), int(c)) for (s, c) in ap.ap][1:]
        name = t.name
        if not pat:
            pat = [(1, 1)]
        if type(t).__name__ == "PSumTensorHandle":
            lo = col
            hi = col
            for (s, c) in pat:
                hi += s * (c - 1)
            return {(name, bnk) for bnk in range((lo * sz) // 2048, (hi * sz) // 2048 + 1)}
        inner_s, inner_c = pat[-1]
        outer = pat[:-1]
        nouter = 1
        for (_, c) in outer:
            nouter *= c
        res = set()
        if nouter > 256:
            lo = col
            hi = col + 1
            for (s, c) in pat:
                hi += s * (c - 1)
            for cc in range((lo * sz) // CELL, (hi * sz - 1) // CELL + 1):
                res.add((name, cc))
            return res
        offs = [col]
        for (s, c) in outer:
            offs = [o + s * i for o in offs for i in range(c)]
        span = inner_s * (inner_c - 1) + 1
        for o in offs:
            for cc in range((o * sz) // CELL, ((o + span) * sz - 1) // CELL + 1):
                res.add((name, cc))
        return res

    def add(self, eng, fn, reads=(), writes=(), dma=False, extra_deps=()):
        op = Op()
        op.eng, op.fn, op.dma, op.sig = eng, fn, dma, False
        op.sigval = op.sem = op.semval = None
        deps = {}
        for d in extra_deps:
            deps[id(d)] = d
        for ap in reads:
            if ap is None or isinstance(ap, (int, float)) or not self._is_chip(ap):
                continue
            for c in self._cells(ap):
                st = self.cells.get(c)
                if st is None:
                    st = self.cells[c] = [None, {}, []]
                if st[0] is not None:
                    deps[id(st[0])] = st[0]
                if dma:
                    st[2].append(op)
                else:
                    st[1][eng] = op
        for ap in writes:
            if ap is None or not self._is_chip(ap):
                continue
            for c in self._cells(ap):
                st = self.cells.get(c)
                if st is None:
                    st = self.cells[c] = [None, {}, []]
                if st[0] is not None:
                    deps[id(st[0])] = st[0]
                for r in st[1].values():
                    deps[id(r)] = r
                for r in st[2]:
                    deps[id(r)] = r
                st[0], st[1], st[2] = op, {}, []
        op.idx = len(self.ops[eng])
        best = {}
        final = []
        for d in deps.values():
            if d is op:
                continue
            if d.dma:
                final.append(d)
                continue
            if d.eng == "pe" and eng == "pe" and not dma:
                continue
            b = best.get(d.eng)
            if b is None or d.idx > b.idx:
                best[d.eng] = d
        for d in best.values():
            d.sig = True
            final.append(d)
        op.deps = final
        if dma:
            k = self.NDMA[eng]
            j = self.dma_n[eng]
            self.dma_n[eng] = j + 1
            op.sem = (eng, j % k)
            op.semval = 16 * (j // k + 1)
        self.ops[eng].append(op)
        return op

    def emit(self, nc, block, sems):
        for eng in self.ENGS:
            cnt = 0
            for op in self.ops[eng]:
                if op.sig and not op.dma:
                    cnt += 1
                    op.sigval = cnt

        def run(eng):
            lst = self.ops[eng]

            def body(e):
                waited = {}
                for op in lst:
                    for d in op.deps:
                        if d.dma:
                            key, val = ("dma",) + d.sem, d.semval
                        else:
                            key, val = ("eng", d.eng), d.sigval
                        if waited.get(key, 0) < val:
                            e.wait_ge(sems[key], val)
                            waited[key] = val
                    if op.dma:
                        key = ("dma",) + op.sem
                        prev = op.semval - 16
                        if prev > 0 and waited.get(key, 0) < prev:
                            e.wait_ge(sems[key], prev)
                            waited[key] = prev
                    if op.fn is None:
                        continue
                    ins = op.fn(e)
                    if op.dma:
                        ins.then_inc(sems[("dma",) + op.sem], 16)
                    elif op.sig:
                        ins.then_inc(sems[("eng", eng)], 1)
            return body

        block.tensor(run("pe"))
        block.scalar(run("act"))
        block.vector(run("dve"))
        block.gpsimd(run("pool"))
        block.sync(run("sp"))


class Arena:
    def __init__(self, ap_f32, words):
        self.base = ap_f32
        self.words = words
        self.off = 0

    def reset(self, off=0):
        self.off = off

    def alloc(self, shape, dtype):
        n = 1
        for s in shape[1:]:
            n *= s
        nbytes = n * DT_SIZE[dtype]
        nbytes = (nbytes + CELL - 1) // CELL * CELL
        w = nbytes // 4
        assert self.off + w <= self.words, ("arena overflow", self.off, w, self.words)
        v = self.base[0:shape[0], self.off:self.off + w]
        self.off += w
        if dtype != F32:
            v = v.bitcast(dtype)
        v = v[:, 0:n]
        if len(shape) == 2:
            return v
        names = "abcdefg"[: len(shape) - 1]
        pat = "p (" + " ".join(names) + ") -> p " + " ".join(names)
        kw = {names[i]: shape[i + 1] for i in range(len(names) - 1)}
        return v.rearrange(pat, **kw)


class _Stop(Exception):
    pass


def build_program(stop=None):
    nc = bass.Bass("TRN2", target_bir_lowering=False)
    dr = {}

    def din(name, shape):
        dr[name] = nc.dram_tensor(name, list(shape), F32, kind="ExternalInput").ap()
        return dr[name]

    x_d = din("x", (TOK, D))
    hm_d = din("hm", (128, 1))
    w_in_d = din("w_in", (DEPTH, D, 2048))
    b_in_d = din("b_in", (DEPTH, 2048))
    vg_d = din("v_ln_g", (DEPTH, 512))
    vb_d = din("v_ln_b", (DEPTH, 512))
    wsp_d = din("w_spatial", (DEPTH, 4, 128, 128))
    bsp_d = din("b_spatial", (DEPTH, 4, 128))
    cw_d = din("conv_w", (DEPTH, CW, 512))
    cb_d = din("conv_b", (DEPTH, 512))
    gg_d = din("gn_g", (DEPTH, 512))
    gb_d = din("gn_b", (DEPTH, 512))
    w_out_d = din("w_out", (DEPTH, D, D))
    b_out_d = din("b_out", (DEPTH, D))
    l1g_d = din("ln1_g", (DEPTH, D))
    l1b_d = din("ln1_b", (DEPTH, D))
    wr_d = din("w_router", (D, NE))
    rb_d = din("router_bias", (NE,))
    wg_d = din("w_gate", (DEPTH, NE, D, 512))
    wu_d = din("w_up", (DEPTH, NE, D, 512))
    wd_d = din("w_down", (DEPTH, NE, 512, D))
    l2g_d = din("ln2_g", (DEPTH, D))
    l2b_d = din("ln2_b", (DEPTH, D))
    out_d = nc.dram_tensor("out", [2048, D], F32, kind="ExternalOutput").ap()

    S = Sched()
    ckpt = [0]

    def chk(name):
        ckpt[0] += 1
        if stop is not None and ckpt[0] == stop:
            print('STOP at', name)
            raise _Stop()
    AW = 101 * 256 - 64

    from contextlib import ExitStack
    with ExitStack() as es:
        def sb(name, shape, dt):
            return es.enter_context(nc.sbuf_tensor(name, list(shape), dt))

        xs = sb("xs", (128, NT, D), F32)
        xT = sb("xT", (128, 8, TOK), BF16)
        ident = sb("ident", (128, 128), F32)
        cmat = sb("cmat", (128, 128), F32)
        odiv = sb("odiv", (128, 128), BF16)
        ones_row = sb("ones_row", (1, 128), F32)
        wr = sb("wr", (128, 8, NE), F32)
        rb_bc = sb("rb_bc", (128, NE), F32)
        hm = sb("hm_sb", (128, 1), F32)
        lg = sb("lg", (128, NT, NE), F32)
        comb = sb("comb", (128, NT, NE), F32)
        lnst_a = sb("lnst", (128, 4, 12), F32)
        lnmv_a = sb("lnmv", (128, 4, 2), F32)
        lnr_a = sb("lnr", (128, 4, 2), F32)
        epst = sb("epst", (128, 1), F32)
        arena_t = sb("arena", (128, AW), F32)
        ps = es.enter_context(nc.psum_tensor("ps", [128, 8, 512], F32))
        A = Arena(arena_t[:, :], AW)

        sems = {}
        for eng in Sched.ENGS:
            sems[("eng", eng)] = es.enter_context(nc.semaphore("s_" + eng))
        for q, k in Sched.NDMA.items():
            for i in range(k):
                sems[("dma", q, i)] = es.enter_context(nc.semaphore("d_%s%d" % (q, i)))

        def mm(out, lhsT, rhs, start, stop, tp=None):
            if tp is None:
                S.add("pe", lambda e: e.matmul(out, lhsT, rhs, start=start, stop=stop),
                      reads=[lhsT, rhs], writes=[out])
            else:
                S.add("pe", lambda e: e.matmul(out, lhsT, rhs, start=start, stop=stop, tile_position=tp),
                      reads=[lhsT, rhs], writes=[out])

        def tr(out, in_, idn):
            S.add("pe", lambda e: e.transpose(out, in_, idn), reads=[in_, idn], writes=[out])

        def act(out, in_, func, bias=None, scale=None, eng="act"):
            kw = {}
            if bias is not None:
                kw["bias"] = bias
            if scale is not None:
                kw["scale"] = scale
            S.add(eng, lambda e: e.activation(out=out, in_=in_, func=func, **kw),
                  reads=[in_, bias, scale], writes=[out])

        def tsc(eng, out, in0, s1, s2, op0, op1=None):
            if op1 is None:
                S.add(eng, lambda e: e.tensor_scalar(out=out, in0=in0, scalar1=s1, scalar2=None, op0=op0),
                      reads=[in0, s1], writes=[out])
            else:
                S.add(eng, lambda e: e.tensor_scalar(out=out, in0=in0, scalar1=s1, scalar2=s2, op0=op0, op1=op1),
                      reads=[in0, s1, s2], writes=[out])

        def stt(eng, out, in0, scalar, in1, op0, op1):
            S.add(eng, lambda e: e.scalar_tensor_tensor(out=out, in0=in0, scalar=scalar, in1=in1, op0=op0, op1=op1),
                  reads=[in0, scalar, in1], writes=[out])

        def tt(eng, out, in0, in1, op):
            S.add(eng, lambda e: e.tensor_tensor(out=out, in0=in0, in1=in1, op=op),
                  reads=[in0, in1], writes=[out])

        def cp(eng, out, in_):
            S.add(eng, lambda e: e.tensor_copy(out=out, in_=in_), reads=[in_], writes=[out])

        def red(eng, out, in_, op):
            S.add(eng, lambda e: e.tensor_reduce(out=out, in_=in_, axis=AX.X, op=op), reads=[in_], writes=[out])

        def dma(q, out, in_):
            return S.add(q, lambda e: e.dma_start(out=out, in_=in_), reads=[in_], writes=[out], dma=True)

        def memset(eng, ap, val):
            S.add(eng, lambda e: e.memset(ap, val), writes=[ap])

        def bank(i, n=512):
            return ps[:, i, 0:n]

        memset("pool", ident[:, :], 1.0)
        S.add("pool", lambda e: e.affine_select(out=ident[:, :], in_=ident[:, :], pattern=[[-1, 128]],
                                                compare_op=ALU.is_ge, fill=0.0, base=0, channel_multiplier=1),
              reads=[ident[:, :]], writes=[ident[:, :]])
        S.add("pool", lambda e: e.affine_select(out=ident[:, :], in_=ident[:, :], pattern=[[1, 128]],
                                                compare_op=ALU.is_ge, fill=0.0, base=0, channel_multiplier=-1),
              reads=[ident[:, :]], writes=[ident[:, :]])
        memset("pool", odiv[:, :], 1.0 / 128.0)
        memset("pool", ones_row[:, :], 1.0)
        memset("pool", epst[:, :], EPS)
        tsc("dve", cmat[:, :], ident[:, :], 1.0 / 128.0, None, ALU.subtract)

        dma("sp", wr[:, :, :], wr_d.rearrange("(k p) e -> p k e", p=128))
        dma("sp", rb_bc[:, :], rb_d.partition_broadcast(128))
        dma("sp", hm[:, :], hm_d)
        xv = x_d.rearrange("(i p) d -> p i d", p=128)
        for (i0, i1) in ((0, 3), (3, 5), (5, 9), (9, 13), (13, 17)):
            dma("sp", xs[:, i0:i1, :], xv[:, i0:i1, :])

        def build_xT(i, tr_bank, lg_ps, xT32, router):
            for h in range(2):
                pb = ps[:, tr_bank, :].rearrange("p (a b) -> p a b", a=4)
                for j in range(4):
                    tr(pb[:, j, :], xs[:, i, (4 * h + j) * 128:(4 * h + j + 1) * 128], ident[:, :])
                yield
                cp("dve", xT[:, 4 * h:4 * h + 4, i * 128:(i + 1) * 128], pb)
                if router:
                    cp("dve", xT32[:, 4 * h:4 * h + 4, :], pb)
                yield
            if router:
                for k in range(8):
                    mm(lg_ps, xT32[:, k, :], wr[:, k, :], k == 0, k == 7)
                yield
                cp("dve", lg[:, i, :], lg_ps)
                yield

        def layer_norm_tile(i, g_bc, b_bc, q=0):
            xt = xs[:, i, :]
            lnst, lnmv, lnr = lnst_a[:, q, :], lnmv_a[:, q, :], lnr_a[:, q, :]
            for c in range(2):
                S.add("dve", lambda e, o=lnst[:, 6 * c:6 * c + 6], s=xs[:, i, c * 512:(c + 1) * 512]: e.bn_stats(out=o, in_=s),
                      reads=[xs[:, i, c * 512:(c + 1) * 512]], writes=[lnst[:, 6 * c:6 * c + 6]])
            yield
            S.add("dve", lambda e: e.bn_aggr(out=lnmv[:, :], in_=lnst[:, :]), reads=[lnst[:, :]], writes=[lnmv[:, :]])
            yield
            act(lnr[:, 0:1], lnmv[:, 1:2], AF.Sqrt, bias=epst[:, 0:1])
            yield
            S.add("dve", lambda e: e.reciprocal(out=lnr[:, 0:1], in_=lnr[:, 0:1]), reads=[lnr[:, 0:1]], writes=[lnr[:, 0:1]])
            tsc("dve", lnr[:, 1:2], lnmv[:, 0:1], lnr[:, 0:1], -1.0, ALU.mult, ALU.mult)
            yield
            act(xt, xt, AF.Identity, bias=lnr[:, 1:2], scale=lnr[:, 0:1])
            yield
            tt("dve", xt, xt, g_bc, ALU.mult)
            yield
            tt("dve", xt, xt, b_bc, ALU.add)
            yield

        def run_threads(ths):
            ths = [t for t in ths if t is not None]
            while ths:
                for t in list(ths):
                    try:
                        next(t)
                    except StopIteration:
                        ths.remove(t)

        def drain(gen):
            for _ in gen:
                pass

        for i in range(NT):
            drain(build_xT(i, 6 + (i % 2), None, None, False))

        out_dmas = []
        try:
          for l in range(DEPTH):
            last = (l == DEPTH - 1)
            A.reset()
            wi = [A.alloc((128, 8, 512), BF16) for _ in range(4)]
            wo = [A.alloc((128, 8, 512), BF16) for _ in range(2)]
            pcol64 = A.alloc((128, 64), F32)
            pcol = pcol64[:, 0:32]
            cbc = pcol64[:, 32:36]
            cwc = A.alloc((128, 124), F32)
            WmT = A.alloc((128, 4, 128), BF16)
            Ch = A.alloc((128, 4, 128), F32)
            bv_bc = A.alloc((128, 512), F32)
            bout_bc = A.alloc((128, D), F32)
            l1g_bc = A.alloc((128, D), F32)
            l1b_bc = A.alloc((128, D), F32)
            xT32 = A.alloc((128, 8, 128), F32)
            NB = 256
            scratch0 = A.off
            guT = A.alloc((128, 4, NB), BF16)
            sig = [A.alloc((128, NB), F32) for _ in range(1)]
            hbufs = [A.alloc((128, 4, 30 + NB), BF16) for _ in range(2)]
            DkH = [A.alloc((128, 16, 128), BF16) for _ in range(2)]
            cacc = [A.alloc((128, NB), F32) for _ in range(2)]
            sqs = [A.alloc((128, NB), BF16) for _ in range(2)]
            yT = [A.alloc((128, 8, NB), BF16) for _ in range(2)]
            t1 = [A.alloc((128, 128), F32)] * 2
            vsm = A.alloc((128, 64), F32)
            vst = vsm[:, 0:24].rearrange('p (a b) -> p a b', a=4)
            vmv = vsm[:, 24:32].rearrange('p (a b) -> p a b', a=4)
            vr = vsm[:, 32:36]
            zv = A.alloc((128, 512), F32)
            gv = zv
            nb = A.alloc((128, 512), BF16)
            A.reset(scratch0)
            stA = A.alloc((128, 128), F32)
            stB = A.alloc((128, 128), F32)
            wsp = A.alloc((128, 4, 128), F32)
            WmT32 = A.alloc((128, 4, 128), F32)
            bcb = A.alloc((128, 512), F32)
            bs_row = A.alloc((1, 4, 128), F32)

            for j in (2, 3, 0, 1):
                dma("pool", wi[j][:, :, :], w_in_d[l, :, j * 512:(j + 1) * 512].rearrange("(k p) f -> p k f", p=128))
            for j in range(2):
                dma("pool", wo[j][:, :, :], w_out_d[l, :, j * 512:(j + 1) * 512].rearrange("(k p) f -> p k f", p=128))
            dma("sp", stA[0:16, :], b_in_d[l].rearrange("(r p) -> r p", p=128))
            dma("sp", stA[16:20, :], cb_d[l].rearrange("(r p) -> r p", p=128))
            dma("sp", stA[20:24, :], gg_d[l].rearrange("(r p) -> r p", p=128))
            dma("sp", stA[24:28, :], gb_d[l].rearrange("(r p) -> r p", p=128))
            dma("sp", stA[28:32, :], vg_d[l].rearrange("(r p) -> r p", p=128))
            dma("sp", stB[0:124, :], cw_d[l].rearrange("k (c p) -> (k c) p", p=128))
            dma("sp", wsp[:, :, :], wsp_d[l].rearrange("h i j -> i h j"))
            dma("sp", bcb[:, :], vb_d[l].partition_broadcast(128))
            dma("sp", bs_row[:, :, :], bsp_d[l].rearrange("(o h) j -> o h j", o=1))
            dma("sp", bv_bc[:, :], b_in_d[l, 512:1024].partition_broadcast(128))
            dma("sp", bout_bc[:, :], b_out_d[l].partition_broadcast(128))
            dma("sp", l1g_bc[:, :], l1g_d[l].partition_broadcast(128))
            dma("sp", l1b_bc[:, :], l1b_d[l].partition_broadcast(128))

            tr(ps[:, 3, 0:32], stA[0:32, :], ident[0:32, 0:32])
            cp("dve", pcol[:, :], ps[:, 3, 0:32])
            tr(ps[:, 4, 0:124], stB[0:124, :], ident[0:124, 0:124])
            cp("dve", cwc[:, :], ps[:, 4, 0:124])
            mm(ps[:, 4, 0:4], cmat[:, :], pcol[:, 16:20], True, True)
            cp("dve", cbc[:, :], ps[:, 4, 0:4])
            S.add("pool", lambda e, w=wsp: e.affine_select(out=w[:, :, :], in_=w[:, :, :], pattern=[[0, 4], [-1, 128]],
                                                            compare_op=ALU.is_ge, fill=0.0, base=0, channel_multiplier=1),
                  reads=[wsp[:, :, :]], writes=[wsp[:, :, :]])
            pb3 = ps[:, 3, :].rearrange("p (a b) -> p a b", a=4)
            for h in range(4):
                tr(pb3[:, h, :], wsp[:, h, :], ident[:, :])
            cp("dve", WmT[:, :, :], pb3)
            cp("dve", WmT32[:, :, :], pb3)
            pb4 = ps[:, 4, :].rearrange("p (a b) -> p a b", a=4)
            for h in range(4):
                mm(pb4[:, h, :], bcb[:, h * 128:(h + 1) * 128], WmT32[:, h, :], True, False)
                mm(pb4[:, h, :], ones_row[0:1, :], bs_row[0:1, h, :], False, True)
            cp("dve", Ch[:, :, :], pb4)

            chk('L%d setup' % l)
            if l == 0:
                blocks = [(0, 128)] + [(128 + NB * b, NB) for b in range(2048 // NB)]
            else:
                blocks = [(0, 128)] + [(128 + NB * b, NB) for b in range(2048 // NB)]
            slot_ctr = [0]

            def pslot(n):
                s = slot_ctr[0] % 2
                slot_ctr[0] += 1
                return ps[:, s, 0:n]

            lg_ps = ps[:, 4, 0:NE]

            nblk = len(blocks)
            F = {"h": [[False] * 4 for _ in range(nblk)], "conv": [[False] * 4 for _ in range(nblk)],
                 "p": [False] * nblk, "a": [False] * nblk, "v": [False] * nblk, "bo": [False] * nblk}

            def is_full(bi):
                return not (blocks[bi][0] == 0 and l > 0)

            def thread_p():
                for bi, (c0, n) in enumerate(blocks):
                    halo = (c0 == 0)
                    hbuf = hbufs[bi % 2]
                    if bi >= 2 and is_full(bi - 2):
                        while not all(F["conv"][bi - 2]):
                            yield
                    if halo:
                        memset("dve", hbuf[:, :, 0:30], 0.0)
                    else:
                        npv = blocks[bi - 1][1]
                        cp("dve", hbuf[:, :, 0:30], hbufs[(bi - 1) % 2][:, :, npv:npv + 30])
                    yield
                    for cc in range(4):
                        pg = ps[:, 0, 0:n]
                        for k in range(8):
                            mm(pg, wi[3][:, k, cc * 128:(cc + 1) * 128], xT[:, k, c0:c0 + n], k == 0, k == 7)
                        yield
                        sg_ = sig[0][:, 0:n]
                        act(sg_, pg, AF.Sigmoid, bias=pcol[:, 12 + cc:13 + cc])
                        yield
                        pa = ps[:, 0, 0:n]
                        for k in range(8):
                            mm(pa, wi[2][:, k, cc * 128:(cc + 1) * 128], xT[:, k, c0:c0 + n], k == 0, k == 7)
                        yield
                        hh = hbuf[:, cc, 30:30 + n]
                        stt("dve", hh, pa, pcol[:, 8 + cc:9 + cc], sg_, ALU.add, ALU.mult)
                        if halo:
                            tsc("dve", hh, hh, hm[:, 0:1], None, ALU.mult)
                        F["h"][bi][cc] = True
                        yield
                    F["p"][bi] = True

            F["acnt"] = [0] * nblk

            def thread_a(tid, ccs, bank_c, bank_v):
                cw3 = cwc[:, :].rearrange("p (k c) -> p k c", c=4)
                Dh = DkH[tid]
                sq = sqs[tid]
                for bi, (c0, n) in enumerate(blocks):
                    if not is_full(bi):
                        F["acnt"][bi] += 1
                        continue
                    hbuf = hbufs[bi % 2]
                    y = yT[bi % 2]
                    while bi >= 2 and is_full(bi - 2) and not F["bo"][bi - 2]:
                        yield
                    for cc in ccs:
                        pc = ps[:, bank_c, 0:n]
                        for (k0, k1) in ((0, 16), (16, CW)):
                            nk = k1 - k0
                            tt("dve", Dh[:, 0:nk, :], cmat[:, :].unsqueeze(1).to_broadcast([128, nk, 128]),
                               cw3[:, k0:k1, cc].unsqueeze(2).to_broadcast([128, nk, 128]), ALU.mult)
                            yield
                            while not F["h"][bi][cc]:
                                yield
                            for k in range(k0, k1):
                                mm(pc, Dh[:, k - k0, :], hbuf[:, cc, k:k + n], k == 0, k == CW - 1)
                                if k % 8 == 7:
                                    yield
                            yield
                        F["conv"][bi][cc] = True
                        vps = ps[:, bank_v, 0:n]
                        act(sq[:, 0:n], pc, AF.Square, bias=cbc[:, cc:cc + 1])
                        yield
                        mm(vps, odiv[:, :], sq[:, 0:n], True, True)
                        yield
                        acc = cacc[tid][:, 0:n]
                        act(acc, vps, AF.Sqrt, bias=epst[:, 0:1])
                        yield
                        S.add("dve", lambda e, o=acc: e.reciprocal(out=o, in_=o), reads=[acc], writes=[acc])
                        yield
                        stt("dve", acc, pc, cbc[:, cc:cc + 1], acc, ALU.add, ALU.mult)
                        yield
                        act(y[:, 4 + cc, 0:n], acc, AF.Silu, bias=pcol[:, 24 + cc:25 + cc], scale=pcol[:, 20 + cc:21 + cc])
                        yield
                    F["acnt"][bi] += 1

            def thread_v_blk(bi, c0, n):
                y = yT[bi % 2]
                for fc in range(4):
                    p = ps[:, 2, 0:n]
                    for k in range(8):
                        mm(p, wi[0][:, k, fc * 128:(fc + 1) * 128], xT[:, k, c0:c0 + n], k == 0, k == 7)
                    yield
                    act(guT[:, fc, 0:n], p, AF.Gelu_apprx_tanh, bias=pcol[:, fc:fc + 1])
                    yield
                for j in range(n // 128):
                    tc0 = c0 + j * 128
                    pv = bank(2)
                    for k in range(8):
                        mm(pv, xT[:, k, tc0:tc0 + 128], wi[1][:, k, :], k == 0, k == 7)
                    yield
                    tt("dve", zv[:, :], pv, bv_bc[:, :], ALU.add)
                    yield
                    act(gv[:, :], zv[:, :], AF.Gelu_apprx_tanh)
                    yield
                    for h in range(4):
                        S.add("dve", lambda e, o=vst[:, h, :], s=gv[:, h * 128:(h + 1) * 128]: e.bn_stats(out=o, in_=s),
                              reads=[gv[:, h * 128:(h + 1) * 128]], writes=[vst[:, h, :]])
                    yield
                    for h in range(4):
                        S.add("dve", lambda e, o=vmv[:, h, :], s=vst[:, h, :]: e.bn_aggr(out=o, in_=s),
                              reads=[vst[:, h, :]], writes=[vmv[:, h, :]])
                    yield
                    act(vr[:, :], vmv[:, :, 1], AF.Sqrt, bias=epst[:, 0:1])
                    yield
                    S.add("dve", lambda e: e.reciprocal(out=vr[:, :], in_=vr[:, :]), reads=[vr[:, :]], writes=[vr[:, :]])
                    yield
                    for h in range(4):
                        tsc("dve", nb[:, h * 128:(h + 1) * 128], gv[:, h * 128:(h + 1) * 128],
                            vmv[:, h, 0:1], vr[:, h:h + 1], ALU.subtract, ALU.mult)
                    yield
                    pm = ps[:, 2, :].rearrange("p (a b) -> p a b", a=4)
                    for h in range(4):
                        mm(pm[:, h, :], nb[:, h * 128:(h + 1) * 128], WmT[:, h, :], True, True)
                    yield
                    for h in range(4):
                        tt_ = t1[h % 2]
                        stt("dve", tt_[:, :], pm[:, h, :], pcol[:, 28 + h:29 + h], Ch[:, h, :], ALU.mult, ALU.add)
                        tt("dve", y[:, h, j * 128:(j + 1) * 128], tt_[:, :], guT[:, h, j * 128:(j + 1) * 128], ALU.mult)
                        yield

            def thread_v():
                for bi, (c0, n) in enumerate(blocks):
                    if is_full(bi):
                        while bi >= 2 and is_full(bi - 2) and not F["bo"][bi - 2]:
                            yield
                        for _ in thread_v_blk(bi, c0, n):
                            yield
                    F["v"][bi] = True

            def thread_b_blk(bi, c0, n):
                y = yT[bi % 2]
                for j in range(n // 128):
                    i = (c0 + j * 128) // 128
                    xt = xs[:, i, :]
                    for hf in range(2):
                        for k in range(8):
                            mm(ps[:, 4 + hf, :], y[:, k, j * 128:(j + 1) * 128], wo[hf][:, k, :], k == 0, k == 7)
                        if j == n // 128 - 1 and hf == 1:
                            F["bo"][bi] = True
                        yield
                    xt2 = xs[:, i, :].rearrange("p (a b) -> p a b", a=2)
                    stt("dve", xt2, xt2, ALPHA, ps[:, 4:6, :], ALU.mult, ALU.add)
                    yield
                    tt("dve", xt, xt, bout_bc[:, :], ALU.add)
                    yield
                    for _ in layer_norm_tile(i, l1g_bc[:, :], l1b_bc[:, :]):
                        yield
                    for _ in build_xT(i, 5, lg_ps, xT32, True):
                        yield
                    act(xt, xt, AF.Identity, scale=ALPHA)
                    yield

            def thread_b():
                for bi, (c0, n) in enumerate(blocks):
                    if not is_full(bi):
                        F["bo"][bi] = True
                        continue
                    while not (F["p"][bi] and F["acnt"][bi] == 2 and F["v"][bi]):
                        yield
                    for _ in thread_b_blk(bi, c0, n):
                        yield

            run_threads([thread_p(), thread_a(0, (0, 2), 1, 7), thread_a(1, (1, 3), 3, 6), thread_v(), thread_b()])
            chk('L%d mixer done' % l)

            A.reset()
            NU = 8
            ring = [A.alloc((128, 8, 512), BF16) for _ in range(NU)]
            hT = [A.alloc((128, 4, 512), BF16) for _ in range(2)]
            sgb = [A.alloc((128, 512), F32) for _ in range(2)]
            l2g_bc = A.alloc((128, D), F32)
            l2b_bc = A.alloc((128, D), F32)
            R = NT * NE
            rt = [A.alloc((128, NT, NE), F32) for _ in range(6)]
            rs1 = [A.alloc((128, NT * 4), F32) for _ in range(4)]
            rs2 = [A.alloc((128, NT), F32) for _ in range(4)]

            dma("sp", l2g_bc[:, :], l2g_d[l].partition_broadcast(128))
            dma("sp", l2b_bc[:, :], l2b_d[l].partition_broadcast(128))

            tiles0 = 0 if l == 0 else 1
            ntl = NT - tiles0
            lgv = lg[:, tiles0:NT, :]

            def bc3(a2, k):
                return a2.unsqueeze(2).to_broadcast([128, a2.shape[1], k])
            ex, sc, sel, e1, sel2, e2 = [r[:, tiles0:NT, :] for r in rt]
            mx, sm, rsm, den = [r[:, tiles0:NT] for r in rs2]
            m1, m2, gsc, geq = [r[:, tiles0 * 4:NT * 4] for r in rs1]
            red("dve", mx, lgv, ALU.max)
            tt("dve", ex, lgv, bc3(mx, NE), ALU.subtract)
            act(ex, ex, AF.Exp)
            red("dve", sm, ex, ALU.add)
            S.add("dve", lambda e, o=rsm, s=sm: e.reciprocal(out=o, in_=s), reads=[sm], writes=[rsm])
            tt("dve", sc, ex, bc3(rsm, NE), ALU.mult)
            tt("dve", sel, sc, rb_bc[:, :].unsqueeze(1).to_broadcast([128, ntl, NE]), ALU.add)
            sel_g = sel.rearrange("p t (g e) -> p (t g) e", e=4)
            e1_g = e1.rearrange("p t (g e) -> p (t g) e", e=4)
            sel2_g = sel2.rearrange("p t (g e) -> p (t g) e", e=4)
            e2_g = e2.rearrange("p t (g e) -> p (t g) e", e=4)
            red("dve", m1, sel_g, ALU.max)
            tt("dve", e1_g, sel_g, bc3(m1, 4), ALU.is_equal)
            stt("dve", sel2_g, e1_g, -1.0e30, sel_g, ALU.mult, ALU.add)
            red("dve", m2, sel2_g, ALU.max)
            tt("dve", e2_g, sel2_g, bc3(m2, 4), ALU.is_equal)
            tt("dve", gsc, m1, m2, ALU.add)
            gsc3 = gsc.rearrange("p (t g) -> p t g", g=4)
            red("dve", den, gsc3, ALU.max)
            geq3 = geq.rearrange("p (t g) -> p t g", g=4)
            tt("dve", geq3, gsc3, bc3(den, 4), ALU.is_equal)
            tt("dve", e1_g, e1_g, e2_g, ALU.add)
            tt("dve", e1_g, e1_g, bc3(geq, 4), ALU.mult)
            tt("dve", sel, e1, sc, ALU.mult)
            red("dve", sm, sel, ALU.add)
            S.add("dve", lambda e, o=rsm, s=sm: e.reciprocal(out=o, in_=s), reads=[sm], writes=[rsm])
            tt("dve", comb[:, tiles0:NT, :], sel, bc3(rsm, NE), ALU.mult)

            chk('L%d routing' % l)
            if l == 0:
                mblocks = [(0, 512), (512, 512), (1024, 384), (1408, 384), (1792, 384)]
            else:
                mblocks = [(128 + 512 * b, 512) for b in range(4)]
            unit_ctr = [0]

            def load_expert(e_):
                us = []
                for (src, pat) in ((wg_d[l, e_], "(k p) f -> p k f"), (wu_d[l, e_], "(k p) f -> p k f")):
                    u = ring[unit_ctr[0] % NU]
                    unit_ctr[0] += 1
                    dma("pool", u[:, :, :], src.rearrange(pat, p=128))
                    us.append(u)
                u = ring[unit_ctr[0] % NU]
                unit_ctr[0] += 1
                ud = u.rearrange("p k f -> p (k f)").rearrange("p (k f) -> p k f", k=4)
                dma("pool", ud, wd_d[l, e_].rearrange("(k p) f -> p k f", p=128))
                us.append(ud)
                return us

            wts = {0: load_expert(0), 1: load_expert(1)}
            work = [(e_, c0, n) for e_ in range(NE) for (c0, n) in mblocks]
            gslot = [0]

            def s1(wi_, idx):
                e_, c0, n = wi_
                Wg, Wu, _ = wts[e_]
                h_ = hT[idx % 2]
                for fc in range(4):
                    s = gslot[0] % 2
                    gslot[0] += 1
                    pg = ps[:, s, 0:n]
                    pu = ps[:, 2 + s, 0:n]
                    for k in range(8):
                        mm(pg, Wg[:, k, fc * 128:(fc + 1) * 128], xT[:, k, c0:c0 + n], k == 0, k == 7)
                    for k in range(8):
                        mm(pu, Wu[:, k, fc * 128:(fc + 1) * 128], xT[:, k, c0:c0 + n], k == 0, k == 7)
                    act(sgb[s][:, 0:n], pg, AF.Silu)
                    tt("dve", h_[:, fc, 0:n], sgb[s][:, 0:n], pu, ALU.mult)

            ytog = [0]

            def s2(wi_, idx):
                e_, c0, n = wi_
                Wd = wts[e_][2]
                h_ = hT[idx % 2]
                for j in range(n // 128):
                    i = (c0 + j * 128) // 128
                    yb = 4 + 2 * (ytog[0] % 2)
                    ytog[0] += 1
                    for hf in range(2):
                        for fc in range(4):
                            mm(ps[:, yb + hf, :], h_[:, fc, j * 128:(j + 1) * 128], Wd[:, fc, hf * 512:(hf + 1) * 512],
                               fc == 0, fc == 3)
                    xt2 = xs[:, i, :].rearrange("p (a b) -> p a b", a=2)
                    stt("dve", xt2, ps[:, yb:yb + 2, :], comb[:, i, e_:e_ + 1], xt2, ALU.mult, ALU.add)
                if (c0, n) == mblocks[-1] and e_ + 2 < NE:
                    wts[e_ + 2] = load_expert(e_ + 2)
                if e_ == NE - 1:
                    def ln2_thread(i, q):
                        for _ in layer_norm_tile(i, l2g_bc[:, :], l2b_bc[:, :], q):
                            yield
                        if not last:
                            for _ in build_xT(i, q, None, None, False):
                                yield
                    tl = [(c0 + j * 128) // 128 for j in range(n // 128)]
                    run_threads([ln2_thread(i, q) for q, i in enumerate(tl)])
                    if last:
                        ov = out_d.rearrange("(i p) d -> p i d", p=128)
                        if tl[0] >= 1:
                            out_dmas.append(dma("sp", ov[:, tl[0] - 1:tl[-1], :], xs[:, tl[0]:tl[-1] + 1, :]))

            for idx, w_ in enumerate(work):
                s1(w_, idx)
                if idx > 0:
                    s2(work[idx - 1], idx - 1)
            s2(work[-1], len(work) - 1)
            chk('L%d experts' % l)

            chk('L%d ln2' % l)
        except _Stop:
            ov = out_d.rearrange("(i p) d -> p i d", p=128)
            for (i0, i1) in ((1, 5), (5, 9), (9, 13), (13, 17)):
                out_dmas.append(dma("sp", ov[:, i0 - 1:i1 - 1, :], xs[:, i0:i1, :]))
        tail_deps = list(out_dmas)
        for _eng in Sched.ENGS:
            _real = [o for o in S.ops[_eng] if o.fn is not None]
            if _real:
                tail_deps.append(_real[-1])
            tail_deps.extend(o for o in S.ops[_eng] if o.dma)
        S.add("sp", None, extra_deps=tail_deps)

        with nc.Block() as block:
            S.emit(nc, block, sems)
    return nc


_NC = None


def kernel(**inputs):
    global _NC
    if _NC is None:
        _NC = build_program()
    x = np.ascontiguousarray(np.asarray(inputs["x"], dtype=np.float32))
    shared = {k: np.ascontiguousarray(np.asarray(v, dtype=np.float32)) for k, v in inputs.items() if k != "x"}
    in_maps = []
    for c in range(NCORES):
        b, q = c // 4, c % 4
        xc = np.zeros((TOK, D), np.float32)
        xc[128:] = x[b, q * 2048:(q + 1) * 2048]
        if q > 0:
            xc[:128] = x[b, q * 2048 - 128:q * 2048]
        m = dict(shared)
        m["x"] = xc
        m["hm"] = np.full((128, 1), 1.0 if q > 0 else 0.0, np.float32)
        in_maps.append(m)
    res = run_bass_kernel_spmd(_NC, in_maps, core_ids=list(range(NCORES)))
    out = np.empty((2, 8192, D), np.float32)
    for c in range(NCORES):
        b, q = c // 4, c % 4
        out[b, q * 2048:(q + 1) * 2048] = res.results[c]["out"]
    return out
```

```python
import numpy as np
import concourse.bass as bass
import concourse.mybir as mybir
from concourse.bass_utils import run_bass_kernel_spmd

F32 = mybir.dt.float32
BF16 = mybir.dt.bfloat16
AF = mybir.ActivationFunctionType
ALU = mybir.AluOpType
AX = mybir.AxisListType

D = 1024
DEPTH = 2
NT = 17
TOK = NT * 128
NE = 16
ALPHA = (2.0 * DEPTH) ** 0.25
EPS = 1e-5
CW = 31
NCORES = 8

DT_SIZE = {F32: 4, BF16: 2}
CELL = 256


class Op:
    __slots__ = ("eng", "fn", "deps", "dma", "idx", "sig", "sigval", "sem", "semval")


class Sched:
    ENGS = ("pe", "act", "dve", "pool", "sp")
    NDMA = {"pool": 12, "sp": 8, "act": 4}

    def __init__(self):
        self.ops = {e: [] for e in self.ENGS}
        self.cells = {}
        self.dma_n = {q: 0 for q in self.NDMA}

    @staticmethod
    def _is_chip(ap):
        return type(ap.tensor).__name__ in ("SBTensorHandle", "PSumTensorHandle")

    def _cells(self, ap):
        t = ap.tensor
        sz = DT_SIZE[ap.dtype]
        shp = list(t.shape)
        row = 1
        for s in shp[1:]:
            row *= int(s)
        col = int(ap.offset) % row
        pat = [(int(s), int(c)) for (s, c) in ap.ap][1:]
        name = t.name
        if not pat:
            pat = [(1, 1)]
        if type(t).__name__ == "PSumTensorHandle":
            lo = col
            hi = col
            for (s, c) in pat:
                hi += s * (c - 1)
            return {(name, bnk) for bnk in range((lo * sz) // 2048, (hi * sz) // 2048 + 1)}
        inner_s, inner_c = pat[-1]
        outer = pat[:-1]
        nouter = 1
        for (_, c) in outer:
            nouter *= c
        res = set()
        if nouter > 256:
            lo = col
            hi = col + 1
            for (s, c) in pat:
                hi += s * (c - 1)
            for cc in range((lo * sz) // CELL, (hi * sz - 1) // CELL + 1):
                res.add((name, cc))
            return res
        offs = [col]
        for (s, c) in outer:
            offs = [o + s * i for o in offs for i in range(c)]
        span = inner_s * (inner_c - 1) + 1
        for o in offs:
            for cc in range((o * sz) // CELL, ((o + span) * sz - 1) // CELL + 1):
                res.add((name, cc))
        return res

    def add(self, eng, fn, reads=(), writes=(), dma=False, extra_deps=()):
        op = Op()
        op.eng, op.fn, op.dma, op.sig = eng, fn, dma, False
        op.sigval = op.sem = op.semval = None
        deps = {}
        for d in extra_deps:
            deps[id(d)] = d
        for ap in reads:
            if ap is None or isinstance(ap, (int, float)) or not self._is_chip(ap):
                continue
            for c in self._cells(ap):
                st = self.cells.get(c)
                if st is None:
                    st = self.cells[c] = [None, {}, []]
                if st[0] is not None:
                    deps[id(st[0])] = st[0]
                if dma:
                    st[2].append(op)
                else:
                    st[1][eng] = op
        for ap in writes:
            if ap is None or not self._is_chip(ap):
                continue
            for c in self._cells(ap):
                st = self.cells.get(c)
                if st is None:
                    st = self.cells[c] = [None, {}, []]
                if st[0] is not None:
                    deps[id(st[0])] = st[0]
                for r in st[1].values():
                    deps[id(r)] = r
                for r in st[2]:
                    deps[id(r)] = r
                st[0], st[1], st[2] = op, {}, []
        op.idx = len(self.ops[eng])
        best = {}
        final = []
        for d in deps.values():
            if d is op:
                continue
            if d.dma:
                final.append(d)
                continue
            if d.eng == "pe" and eng == "pe" and not dma:
                continue
            b = best.get(d.eng)
            if b is None or d.idx > b.idx:
                best[d.eng] = d
        for d in best.values():
            d.sig = True
            final.append(d)
        op.deps = final
        if dma:
            k = self.NDMA[eng]
            j = self.dma_n[eng]
            self.dma_n[eng] = j + 1
            op.sem = (eng, j % k)
            op.semval = 16 * (j // k + 1)
        self.ops[eng].append(op)
        return op

    def emit(self, nc, block, sems):
        for eng in self.ENGS:
            cnt = 0
            for op in self.ops[eng]:
                if op.sig and not op.dma:
                    cnt += 1
                    op.sigval = cnt

        def run(eng):
            lst = self.ops[eng]

            def body(e):
                waited = {}
                for op in lst:
                    for d in op.deps:
                        if d.dma:
                            key, val = ("dma",) + d.sem, d.semval
                        else:
                            key, val = ("eng", d.eng), d.sigval
                        if waited.get(key, 0) < val:
                            e.wait_ge(sems[key], val)
                            waited[key] = val
                    if op.dma:
                        key = ("dma",) + op.sem
                        prev = op.semval - 16
                        if prev > 0 and waited.get(key, 0) < prev:
                            e.wait_ge(sems[key], prev)
                            waited[key] = prev
                    if op.fn is None:
                        continue
                    ins = op.fn(e)
                    if op.dma:
                        ins.then_inc(sems[("dma",) + op.sem], 16)
                    elif op.sig:
                        ins.then_inc(sems[("eng", eng)], 1)
            return body

        block.tensor(run("pe"))
        block.scalar(run("act"))
        block.vector(run("dve"))
        block.gpsimd(run("pool"))
        block.sync(run("sp"))


class Arena:
    def __init__(self, ap_f32, words):
        self.base = ap_f32
        self.words = words
        self.off = 0

    def reset(self, off=0):
        self.off = off

    def alloc(self, shape, dtype):
        n = 1
        for s in shape[1:]:
            n *= s
        nbytes = n * DT_SIZE[dtype]
        nbytes = (nbytes + CELL - 1) // CELL * CELL
        w = nbytes // 4
        assert self.off + w <= self.words, ("arena overflow", self.off, w, self.words)
        v = self.base[0:shape[0], self.off:self.off + w]
        self.off += w
        if dtype != F32:
            v = v.bitcast(dtype)
        v = v[:, 0:n]
        if len(shape) == 2:
            return v
        names = "abcdefg"[: len(shape) - 1]
        pat = "p (" + " ".join(names) + ") -> p " + " ".join(names)
        kw = {names[i]: shape[i + 1] for i in range(len(names) - 1)}
        return v.rearrange(pat, **kw)


class _Stop(Exception):
    pass


def build_program(stop=None):
    nc = bass.Bass("TRN2", target_bir_lowering=False)
    dr = {}

    def din(name, shape):
        dr[name] = nc.dram_tensor(name, list(shape), F32, kind="ExternalInput").ap()
        return dr[name]

    x_d = din("x", (TOK, D))
    hm_d = din("hm", (128, 1))
    w_in_d = din("w_in", (DEPTH, D, 2048))
    b_in_d = din("b_in", (DEPTH, 2048))
    vg_d = din("v_ln_g", (DEPTH, 512))
    vb_d = din("v_ln_b", (DEPTH, 512))
    wsp_d = din("w_spatial", (DEPTH, 4, 128, 128))
    bsp_d = din("b_spatial", (DEPTH, 4, 128))
    cw_d = din("conv_w", (DEPTH, CW, 512))
    cb_d = din("conv_b", (DEPTH, 512))
    gg_d = din("gn_g", (DEPTH, 512))
    gb_d = din("gn_b", (DEPTH, 512))
    w_out_d = din("w_out", (DEPTH, D, D))
    b_out_d = din("b_out", (DEPTH, D))
    l1g_d = din("ln1_g", (DEPTH, D))
    l1b_d = din("ln1_b", (DEPTH, D))
    wr_d = din("w_router", (D, NE))
    rb_d = din("router_bias", (NE,))
    wg_d = din("w_gate", (DEPTH, NE, D, 512))
    wu_d = din("w_up", (DEPTH, NE, D, 512))
    wd_d = din("w_down", (DEPTH, NE, 512, D))
    l2g_d = din("ln2_g", (DEPTH, D))
    l2b_d = din("ln2_b", (DEPTH, D))
    out_d = nc.dram_tensor("out", [2048, D], F32, kind="ExternalOutput").ap()

    S = Sched()
    ckpt = [0]

    def chk(name):
        ckpt[0] += 1
        if stop is not None and ckpt[0] == stop:
            print('STOP at', name)
            raise _Stop()
    AW = 101 * 256 - 64

    from contextlib import ExitStack
    with ExitStack() as es:
        def sb(name, shape, dt):
            return es.enter_context(nc.sbuf_tensor(name, list(shape), dt))

        xs = sb("xs", (128, NT, D), F32)
        xT = sb("xT", (128, 8, TOK), BF16)
        ident = sb("ident", (128, 128), F32)
        cmat = sb("cmat", (128, 128), F32)
        odiv = sb("odiv", (128, 128), BF16)
        ones_row = sb("ones_row", (1, 128), F32)
        wr = sb("wr", (128, 8, NE), F32)
        rb_bc = sb("rb_bc", (128, NE), F32)
        hm = sb("hm_sb", (128, 1), F32)
        lg = sb("lg", (128, NT, NE), F32)
        comb = sb("comb", (128, NT, NE), F32)
        lnst_a = sb("lnst", (128, 4, 12), F32)
        lnmv_a = sb("lnmv", (128, 4, 2), F32)
        lnr_a = sb("lnr", (128, 4, 2), F32)
        epst = sb("epst", (128, 1), F32)
        arena_t = sb("arena", (128, AW), F32)
        ps = es.enter_context(nc.psum_tensor("ps", [128, 8, 512], F32))
        A = Arena(arena_t[:, :], AW)

        sems = {}
        for eng in Sched.ENGS:
            sems[("eng", eng)] = es.enter_context(nc.semaphore("s_" + eng))
        for q, k in Sched.NDMA.items():
            for i in range(k):
                sems[("dma", q, i)] = es.enter_context(nc.semaphore("d_%s%d" % (q, i)))

        def mm(out, lhsT, rhs, start, stop, tp=None):
            if tp is None:
                S.add("pe", lambda e: e.matmul(out, lhsT, rhs, start=start, stop=stop),
                      reads=[lhsT, rhs], writes=[out])
            else:
                S.add("pe", lambda e: e.matmul(out, lhsT, rhs, start=start, stop=stop, tile_position=tp),
                      reads=[lhsT, rhs], writes=[out])

        def tr(out, in_, idn):
            S.add("pe", lambda e: e.transpose(out, in_, idn), reads=[in_, idn], writes=[out])

        def act(out, in_, func, bias=None, scale=None, eng="act"):
            kw = {}
            if bias is not None:
                kw["bias"] = bias
            if scale is not None:
                kw["scale"] = scale
            S.add(eng, lambda e: e.activation(out=out, in_=in_, func=func, **kw),
                  reads=[in_, bias, scale], writes=[out])

        def tsc(eng, out, in0, s1, s2, op0, op1=None):
            if op1 is None:
                S.add(eng, lambda e: e.tensor_scalar(out=out, in0=in0, scalar1=s1, scalar2=None, op0=op0),
                      reads=[in0, s1], writes=[out])
            else:
                S.add(eng, lambda e: e.tensor_scalar(out=out, in0=in0, scalar1=s1, scalar2=s2, op0=op0, op1=op1),
                      reads=[in0, s1, s2], writes=[out])

        def stt(eng, out, in0, scalar, in1, op0, op1):
            S.add(eng, lambda e: e.scalar_tensor_tensor(out=out, in0=in0, scalar=scalar, in1=in1, op0=op0, op1=op1),
                  reads=[in0, scalar, in1], writes=[out])

        def tt(eng, out, in0, in1, op):
            S.add(eng, lambda e: e.tensor_tensor(out=out, in0=in0, in1=in1, op=op),
                  reads=[in0, in1], writes=[out])

        def cp(eng, out, in_):
            S.add(eng, lambda e: e.tensor_copy(out=out, in_=in_), reads=[in_], writes=[out])

        def red(eng, out, in_, op):
            S.add(eng, lambda e: e.tensor_reduce(out=out, in_=in_, axis=AX.X, op=op), reads=[in_], writes=[out])

        def dma(q, out, in_):
            return S.add(q, lambda e: e.dma_start(out=out, in_=in_), reads=[in_], writes=[out], dma=True)

        def memset(eng, ap, val):
            S.add(eng, lambda e: e.memset(ap, val), writes=[ap])

        def bank(i, n=512):
            return ps[:, i, 0:n]

        memset("pool", ident[:, :], 1.0)
        S.add("pool", lambda e: e.affine_select(out=ident[:, :], in_=ident[:, :], pattern=[[-1, 128]],
                                                compare_op=ALU.is_ge, fill=0.0, base=0, channel_multiplier=1),
              reads=[ident[:, :]], writes=[ident[:, :]])
        S.add("pool", lambda e: e.affine_select(out=ident[:, :], in_=ident[:, :], pattern=[[1, 128]],
                                                compare_op=ALU.is_ge, fill=0.0, base=0, channel_multiplier=-1),
              reads=[ident[:, :]], writes=[ident[:, :]])
        memset("pool", odiv[:, :], 1.0 / 128.0)
        memset("pool", ones_row[:, :], 1.0)
        memset("pool", epst[:, :], EPS)
        tsc("dve", cmat[:, :], ident[:, :], 1.0 / 128.0, None, ALU.subtract)

        dma("sp", wr[:, :, :], wr_d.rearrange("(k p) e -> p k e", p=128))
        dma("sp", rb_bc[:, :], rb_d.partition_broadcast(128))
        dma("sp", hm[:, :], hm_d)
        xv = x_d.rearrange("(i p) d -> p i d", p=128)
        for (i0, i1) in ((0, 3), (3, 5), (5, 9), (9, 13), (13, 17)):
            dma("sp", xs[:, i0:i1, :], xv[:, i0:i1, :])

        def build_xT(i, tr_bank, lg_ps, xT32, router):
            for h in range(2):
                pb = ps[:, tr_bank, :].rearrange("p (a b) -> p a b", a=4)
                for j in range(4):
                    tr(pb[:, j, :], xs[:, i, (4 * h + j) * 128:(4 * h + j + 1) * 128], ident[:, :])
                yield
                cp("dve", xT[:, 4 * h:4 * h + 4, i * 128:(i + 1) * 128], pb)
                if router:
                    cp("dve", xT32[:, 4 * h:4 * h + 4, :], pb)
                yield
            if router:
                for k in range(8):
                    mm(lg_ps, xT32[:, k, :], wr[:, k, :], k == 0, k == 7)
                yield
                cp("dve", lg[:, i, :], lg_ps)
                yield

        def layer_norm_tile(i, g_bc, b_bc, q=0):
            xt = xs[:, i, :]
            lnst, lnmv, lnr = lnst_a[:, q, :], lnmv_a[:, q, :], lnr_a[:, q, :]
            for c in range(2):
                S.add("dve", lambda e, o=lnst[:, 6 * c:6 * c + 6], s=xs[:, i, c * 512:(c + 1) * 512]: e.bn_stats(out=o, in_=s),
                      reads=[xs[:, i, c * 512:(c + 1) * 512]], writes=[lnst[:, 6 * c:6 * c + 6]])
            yield
            S.add("dve", lambda e: e.bn_aggr(out=lnmv[:, :], in_=lnst[:, :]), reads=[lnst[:, :]], writes=[lnmv[:, :]])
            yield
            act(lnr[:, 0:1], lnmv[:, 1:2], AF.Sqrt, bias=epst[:, 0:1])
            yield
            S.add("dve", lambda e: e.reciprocal(out=lnr[:, 0:1], in_=lnr[:, 0:1]), reads=[lnr[:, 0:1]], writes=[lnr[:, 0:1]])
            tsc("dve", lnr[:, 1:2], lnmv[:, 0:1], lnr[:, 0:1], -1.0, ALU.mult, ALU.mult)
            yield
            act(xt, xt, AF.Identity, bias=lnr[:, 1:2], scale=lnr[:, 0:1])
            yield
            tt("dve", xt, xt, g_bc, ALU.mult)
            yield
            tt("dve", xt, xt, b_bc, ALU.add)
            yield

        def run_threads(ths):
            ths = [t for t in ths if t is not None]
            while ths:
                for t in list(ths):
                    try:
                        next(t)
                    except StopIteration:
                        ths.remove(t)

        def drain(gen):
            for _ in gen:
                pass

        for i in range(NT):
            drain(build_xT(i, 6 + (i % 2), None, None, False))

        out_dmas = []
        try:
          for l in range(DEPTH):
            last = (l == DEPTH - 1)
            A.reset()
            wi = [A.alloc((128, 8, 512), BF16) for _ in range(4)]
            wo = [A.alloc((128, 8, 512), BF16) for _ in range(2)]
            pcol64 = A.alloc((128, 64), F32)
            pcol = pcol64[:, 0:32]
            cbc = pcol64[:, 32:36]
            cwc = A.alloc((128, 124), F32)
            WmT = A.alloc((128, 4, 128), BF16)
            Ch = A.alloc((128, 4, 128), F32)
            bv_bc = A.alloc((128, 512), F32)
            bout_bc = A.alloc((128, D), F32)
            l1g_bc = A.alloc((128, D), F32)
            l1b_bc = A.alloc((128, D), F32)
            xT32 = A.alloc((128, 8, 128), F32)
            NB = 256
            scratch0 = A.off
            guT = A.alloc((128, 4, NB), BF16)
            sig = [A.alloc((128, NB), F32) for _ in range(1)]
            hbufs = [A.alloc((128, 4, 30 + NB), BF16) for _ in range(2)]
            DkH = [A.alloc((128, 16, 128), BF16) for _ in range(2)]
            cacc = [A.alloc((128, NB), F32) for _ in range(2)]
            sqs = [A.alloc((128, NB), BF16) for _ in range(2)]
            yT = [A.alloc((128, 8, NB), BF16) for _ in range(2)]
            t1 = [A.alloc((128, 128), F32)] * 2
            vsm = A.alloc((128, 64), F32)
            vst = vsm[:, 0:24].rearrange('p (a b) -> p a b', a=4)
            vmv = vsm[:, 24:32].rearrange('p (a b) -> p a b', a=4)
            vr = vsm[:, 32:36]
            zv = A.alloc((128, 512), F32)
            gv = zv
            nb = A.alloc((128, 512), BF16)
            A.reset(scratch0)
            stA = A.alloc((128, 128), F32)
            stB = A.alloc((128, 128), F32)
            wsp = A.alloc((128, 4, 128), F32)
            WmT32 = A.alloc((128, 4, 128), F32)
            bcb = A.alloc((128, 512), F32)
            bs_row = A.alloc((1, 4, 128), F32)

            for j in (2, 3, 0, 1):
                dma("pool", wi[j][:, :, :], w_in_d[l, :, j * 512:(j + 1) * 512].rearrange("(k p) f -> p k f", p=128))
            for j in range(2):
                dma("pool", wo[j][:, :, :], w_out_d[l, :, j * 512:(j + 1) * 512].rearrange("(k p) f -> p k f", p=128))
            dma("sp", stA[0:16, :], b_in_d[l].rearrange("(r p) -> r p", p=128))
            dma("sp", stA[16:20, :], cb_d[l].rearrange("(r p) -> r p", p=128))
            dma("sp", stA[20:24, :], gg_d[l].rearrange("(r p) -> r p", p=128))
            dma("sp", stA[24:28, :], gb_d[l].rearrange("(r p) -> r p", p=128))
            dma("sp", stA[28:32, :], vg_d[l].rearrange("(r p) -> r p", p=128))
            dma("sp", stB[0:124, :], cw_d[l].rearrange("k (c p) -> (k c) p", p=128))
            dma("sp", wsp[:, :, :], wsp_d[l].rearrange("h i j -> i h j"))
            dma("sp", bcb[:, :], vb_d[l].partition_broadcast(128))
            dma("sp", bs_row[:, :, :], bsp_d[l].rearrange("(o h) j -> o h j", o=1))
            dma("sp", bv_bc[:, :], b_in_d[l, 512:1024].partition_broadcast(128))
            dma("sp", bout_bc[:, :], b_out_d[l].partition_broadcast(128))
            dma("sp", l1g_bc[:, :], l1g_d[l].partition_broadcast(128))
            dma("sp", l1b_bc[:, :], l1b_d[l].partition_broadcast(128))

            tr(ps[:, 3, 0:32], stA[0:32, :], ident[0:32, 0:32])
            cp("dve", pcol[:, :], ps[:, 3, 0:32])
            tr(ps[:, 4, 0:124], stB[0:124, :], ident[0:124, 0:124])
            cp("dve", cwc[:, :], ps[:, 4, 0:124])
            mm(ps[:, 4, 0:4], cmat[:, :], pcol[:, 16:20], True, True)
            cp("dve", cbc[:, :], ps[:, 4, 0:4])
            S.add("pool", lambda e, w=wsp: e.affine_select(out=w[:, :, :], in_=w[:, :, :], pattern=[[0, 4], [-1, 128]],
                                                            compare_op=ALU.is_ge, fill=0.0, base=0, channel_multiplier=1),
                  reads=[wsp[:, :, :]], writes=[wsp[:, :, :]])
            pb3 = ps[:, 3, :].rearrange("p (a b) -> p a b", a=4)
            for h in range(4):
                tr(pb3[:, h, :], wsp[:, h, :], ident[:, :])
            cp("dve", WmT[:, :, :], pb3)
            cp("dve", WmT32[:, :, :], pb3)
            pb4 = ps[:, 4, :].rearrange("p (a b) -> p a b", a=4)
            for h in range(4):
                mm(pb4[:, h, :], bcb[:, h * 128:(h + 1) * 128], WmT32[:, h, :], True, False)
                mm(pb4[:, h, :], ones_row[0:1, :], bs_row[0:1, h, :], False, True)
            cp("dve", Ch[:, :, :], pb4)

            chk('L%d setup' % l)
            if l == 0:
                blocks = [(0, 128)] + [(128 + NB * b, NB) for b in range(2048 // NB)]
            else:
                blocks = [(0, 128)] + [(128 + NB * b, NB) for b in range(2048 // NB)]
            slot_ctr = [0]

            def pslot(n):
                s = slot_ctr[0] % 2
                slot_ctr[0] += 1
                return ps[:, s, 0:n]

            lg_ps = ps[:, 4, 0:NE]

            nblk = len(blocks)
            F = {"h": [[False] * 4 for _ in range(nblk)], "conv": [[False] * 4 for _ in range(nblk)],
                 "p": [False] * nblk, "a": [False] * nblk, "v": [False] * nblk, "bo": [False] * nblk}

            def is_full(bi):
                return not (blocks[bi][0] == 0 and l > 0)

            def thread_p():
                for bi, (c0, n) in enumerate(blocks):
                    halo = (c0 == 0)
                    hbuf = hbufs[bi % 2]
                    if bi >= 2 and is_full(bi - 2):
                        while not all(F["conv"][bi - 2]):
                            yield
                    if halo:
                        memset("dve", hbuf[:, :, 0:30], 0.0)
                    else:
                        npv = blocks[bi - 1][1]
                        cp("dve", hbuf[:, :, 0:30], hbufs[(bi - 1) % 2][:, :, npv:npv + 30])
                    yield
                    for cc in range(4):
                        pg = ps[:, 0, 0:n]
                        pa = ps[:, 0, 256:256 + n]
                        for k in range(8):
                            mm(pg, wi[3][:, k, cc * 128:(cc + 1) * 128], xT[:, k, c0:c0 + n], k == 0, k == 7)
                        for k in range(8):
                            mm(pa, wi[2][:, k, cc * 128:(cc + 1) * 128], xT[:, k, c0:c0 + n], k == 0, k == 7)
                        yield
                        sg_ = sig[0][:, 0:n]
                        act(sg_, pg, AF.Sigmoid, bias=pcol[:, 12 + cc:13 + cc])
                        yield
                        hh = hbuf[:, cc, 30:30 + n]
                        stt("dve", hh, pa, pcol[:, 8 + cc:9 + cc], sg_, ALU.add, ALU.mult)
                        if halo:
                            tsc("dve", hh, hh, hm[:, 0:1], None, ALU.mult)
                        F["h"][bi][cc] = True
                        yield
                    F["p"][bi] = True

            F["acnt"] = [0] * nblk

            def thread_a(tid, ccs, bank_c, bank_v):
                cw3 = cwc[:, :].rearrange("p (k c) -> p k c", c=4)
                Dh = DkH[tid]
                sq = sqs[tid]
                for bi, (c0, n) in enumerate(blocks):
                    if not is_full(bi):
                        F["acnt"][bi] += 1
                        continue
                    hbuf = hbufs[bi % 2]
                    y = yT[bi % 2]
                    while bi >= 2 and is_full(bi - 2) and not F["bo"][bi - 2]:
                        yield
                    for cc in ccs:
                        pc = ps[:, bank_c, 0:n]
                        for (k0, k1) in ((0, 16), (16, CW)):
                            nk = k1 - k0
                            tt("dve", Dh[:, 0:nk, :], cmat[:, :].unsqueeze(1).to_broadcast([128, nk, 128]),
                               cw3[:, k0:k1, cc].unsqueeze(2).to_broadcast([128, nk, 128]), ALU.mult)
                            yield
                            while not F["h"][bi][cc]:
                                yield
                            for k in range(k0, k1):
                                mm(pc, Dh[:, k - k0, :], hbuf[:, cc, k:k + n], k == 0, k == CW - 1)
                                if k % 8 == 7:
                                    yield
                            yield
                        F["conv"][bi][cc] = True
                        vps = ps[:, bank_v, 0:n]
                        act(sq[:, 0:n], pc, AF.Square, bias=cbc[:, cc:cc + 1])
                        yield
                        mm(vps, odiv[:, :], sq[:, 0:n], True, True)
                        yield
                        acc = cacc[tid][:, 0:n]
                        act(acc, vps, AF.Sqrt, bias=epst[:, 0:1])
                        yield
                        S.add("dve", lambda e, o=acc: e.reciprocal(out=o, in_=o), reads=[acc], writes=[acc])
                        yield
                        stt("dve", acc, pc, cbc[:, cc:cc + 1], acc, ALU.add, ALU.mult)
                        yield
                        act(y[:, 4 + cc, 0:n], acc, AF.Silu, bias=pcol[:, 24 + cc:25 + cc], scale=pcol[:, 20 + cc:21 + cc])
                        yield
                    F["acnt"][bi] += 1

            def thread_v_blk(bi, c0, n):
                y = yT[bi % 2]
                for f2 in range(2):
                    pp = [ps[:, 2, 0:n], ps[:, 2, 256:256 + n]]
                    for q in range(2):
                        fc = 2 * f2 + q
                        for k in range(8):
                            mm(pp[q], wi[0][:, k, fc * 128:(fc + 1) * 128], xT[:, k, c0:c0 + n], k == 0, k == 7)
                    yield
                    for q in range(2):
                        fc = 2 * f2 + q
                        act(guT[:, fc, 0:n], pp[q], AF.Gelu_apprx_tanh, bias=pcol[:, fc:fc + 1])
                    yield
                for j in range(n // 128):
                    tc0 = c0 + j * 128
                    pv = bank(2)
                    for k in range(8):
                        mm(pv, xT[:, k, tc0:tc0 + 128], wi[1][:, k, :], k == 0, k == 7)
                    yield
                    tt("dve", zv[:, :], pv, bv_bc[:, :], ALU.add)
                    yield
                    act(gv[:, :], zv[:, :], AF.Gelu_apprx_tanh)
                    yield
                    for h in range(4):
                        S.add("dve", lambda e, o=vst[:, h, :], s=gv[:, h * 128:(h + 1) * 128]: e.bn_stats(out=o, in_=s),
                              reads=[gv[:, h * 128:(h + 1) * 128]], writes=[vst[:, h, :]])
                    yield
                    for h in range(4):
                        S.add("dve", lambda e, o=vmv[:, h, :], s=vst[:, h, :]: e.bn_aggr(out=o, in_=s),
                              reads=[vst[:, h, :]], writes=[vmv[:, h, :]])
                    yield
                    act(vr[:, :], vmv[:, :, 1], AF.Sqrt, bias=epst[:, 0:1])
                    yield
                    S.add("dve", lambda e: e.reciprocal(out=vr[:, :], in_=vr[:, :]), reads=[vr[:, :]], writes=[vr[:, :]])
                    yield
                    for h in range(4):
                        tsc("dve", nb[:, h * 128:(h + 1) * 128], gv[:, h * 128:(h + 1) * 128],
                            vmv[:, h, 0:1], vr[:, h:h + 1], ALU.subtract, ALU.mult)
                    yield
                    pm = ps[:, 2, :].rearrange("p (a b) -> p a b", a=4)
                    for h in range(4):
                        mm(pm[:, h, :], nb[:, h * 128:(h + 1) * 128], WmT[:, h, :], True, True)
                    yield
                    for h in range(4):
                        tt_ = t1[h % 2]
                        stt("dve", tt_[:, :], pm[:, h, :], pcol[:, 28 + h:29 + h], Ch[:, h, :], ALU.mult, ALU.add)
                        tt("dve", y[:, h, j * 128:(j + 1) * 128], tt_[:, :], guT[:, h, j * 128:(j + 1) * 128], ALU.mult)
                        yield

            def thread_v():
                for bi, (c0, n) in enumerate(blocks):
                    if is_full(bi):
                        while bi >= 2 and is_full(bi - 2) and not F["bo"][bi - 2]:
                            yield
                        for _ in thread_v_blk(bi, c0, n):
                            yield
                    F["v"][bi] = True

            def thread_b_blk(bi, c0, n):
                y = yT[bi % 2]
                for j in range(n // 128):
                    i = (c0 + j * 128) // 128
                    xt = xs[:, i, :]
                    for hf in range(2):
                        for k in range(8):
                            mm(ps[:, 4 + hf, :], y[:, k, j * 128:(j + 1) * 128], wo[hf][:, k, :], k == 0, k == 7)
                        if j == n // 128 - 1 and hf == 1:
                            F["bo"][bi] = True
                        yield
                    xt2 = xs[:, i, :].rearrange("p (a b) -> p a b", a=2)
                    stt("dve", xt2, xt2, ALPHA, ps[:, 4:6, :], ALU.mult, ALU.add)
                    yield
                    tt("dve", xt, xt, bout_bc[:, :], ALU.add)
                    yield
                    for _ in layer_norm_tile(i, l1g_bc[:, :], l1b_bc[:, :]):
                        yield
                    for _ in build_xT(i, 5, lg_ps, xT32, True):
                        yield
                    act(xt, xt, AF.Identity, scale=ALPHA)
                    yield

            def thread_b():
                for bi, (c0, n) in enumerate(blocks):
                    if not is_full(bi):
                        F["bo"][bi] = True
                        continue
                    while not (F["p"][bi] and F["acnt"][bi] == 2 and F["v"][bi]):
                        yield
                    for _ in thread_b_blk(bi, c0, n):
                        yield

            run_threads([thread_p(), thread_a(0, (0, 2), 1, 7), thread_a(1, (1, 3), 3, 6), thread_v(), thread_b()])
            chk('L%d mixer done' % l)

            A.reset()
            NU = 8
            ring = [A.alloc((128, 8, 512), BF16) for _ in range(NU)]
            hT = [A.alloc((128, 4, 512), BF16) for _ in range(2)]
            sgb = [A.alloc((128, 512), F32) for _ in range(2)]
            l2g_bc = A.alloc((128, D), F32)
            l2b_bc = A.alloc((128, D), F32)
            R = NT * NE
            rt = [A.alloc((128, NT, NE), F32) for _ in range(6)]
            rs1 = [A.alloc((128, NT * 4), F32) for _ in range(4)]
            rs2 = [A.alloc((128, NT), F32) for _ in range(4)]

            dma("sp", l2g_bc[:, :], l2g_d[l].partition_broadcast(128))
            dma("sp", l2b_bc[:, :], l2b_d[l].partition_broadcast(128))

            tiles0 = 0 if l == 0 else 1
            ntl = NT - tiles0
            lgv = lg[:, tiles0:NT, :]

            def bc3(a2, k):
                return a2.unsqueeze(2).to_broadcast([128, a2.shape[1], k])
            ex, sc, sel, e1, sel2, e2 = [r[:, tiles0:NT, :] for r in rt]
            mx, sm, rsm, den = [r[:, tiles0:NT] for r in rs2]
            m1, m2, gsc, geq = [r[:, tiles0 * 4:NT * 4] for r in rs1]
            red("dve", mx, lgv, ALU.max)
            tt("dve", ex, lgv, bc3(mx, NE), ALU.subtract)
            act(ex, ex, AF.Exp)
            red("dve", sm, ex, ALU.add)
            S.add("dve", lambda e, o=rsm, s=sm: e.reciprocal(out=o, in_=s), reads=[sm], writes=[rsm])
            tt("dve", sc, ex, bc3(rsm, NE), ALU.mult)
            tt("dve", sel, sc, rb_bc[:, :].unsqueeze(1).to_broadcast([128, ntl, NE]), ALU.add)
            sel_g = sel.rearrange("p t (g e) -> p (t g) e", e=4)
            e1_g = e1.rearrange("p t (g e) -> p (t g) e", e=4)
            sel2_g = sel2.rearrange("p t (g e) -> p (t g) e", e=4)
            e2_g = e2.rearrange("p t (g e) -> p (t g) e", e=4)
            red("dve", m1, sel_g, ALU.max)
            tt("dve", e1_g, sel_g, bc3(m1, 4), ALU.is_equal)
            stt("dve", sel2_g, e1_g, -1.0e30, sel_g, ALU.mult, ALU.add)
            red("dve", m2, sel2_g, ALU.max)
            tt("dve", e2_g, sel2_g, bc3(m2, 4), ALU.is_equal)
            tt("dve", gsc, m1, m2, ALU.add)
            gsc3 = gsc.rearrange("p (t g) -> p t g", g=4)
            red("dve", den, gsc3, ALU.max)
            geq3 = geq.rearrange("p (t g) -> p t g", g=4)
            tt("dve", geq3, gsc3, bc3(den, 4), ALU.is_equal)
            tt("dve", e1_g, e1_g, e2_g, ALU.add)
            tt("dve", e1_g, e1_g, bc3(geq, 4), ALU.mult)
            tt("dve", sel, e1, sc, ALU.mult)
            red("dve", sm, sel, ALU.add)
            S.add("dve", lambda e, o=rsm, s=sm: e.reciprocal(out=o, in_=s), reads=[sm], writes=[rsm])
            tt("dve", comb[:, tiles0:NT, :], sel, bc3(rsm, NE), ALU.mult)

            chk('L%d routing' % l)
            if l == 0:
                mblocks = [(0, 512), (512, 512), (1024, 384), (1408, 384), (1792, 384)]
            else:
                mblocks = [(128 + 512 * b, 512) for b in range(4)]
            unit_ctr = [0]

            def load_expert(e_):
                us = []
                for (src, pat) in ((wg_d[l, e_], "(k p) f -> p k f"), (wu_d[l, e_], "(k p) f -> p k f")):
                    u = ring[unit_ctr[0] % NU]
                    unit_ctr[0] += 1
                    dma("pool", u[:, :, :], src.rearrange(pat, p=128))
                    us.append(u)
                u = ring[unit_ctr[0] % NU]
                unit_ctr[0] += 1
                ud = u.rearrange("p k f -> p (k f)").rearrange("p (k f) -> p k f", k=4)
                dma("pool", ud, wd_d[l, e_].rearrange("(k p) f -> p k f", p=128))
                us.append(ud)
                return us

            wts = {0: load_expert(0), 1: load_expert(1)}
            work = [(e_, c0, n) for e_ in range(NE) for (c0, n) in mblocks]
            gslot = [0]

            def s1(wi_, idx):
                e_, c0, n = wi_
                Wg, Wu, _ = wts[e_]
                h_ = hT[idx % 2]
                for fc in range(4):
                    s = gslot[0] % 2
                    gslot[0] += 1
                    pg = ps[:, s, 0:n]
                    pu = ps[:, 2 + s, 0:n]
                    for k in range(8):
                        mm(pg, Wg[:, k, fc * 128:(fc + 1) * 128], xT[:, k, c0:c0 + n], k == 0, k == 7)
                    for k in range(8):
                        mm(pu, Wu[:, k, fc * 128:(fc + 1) * 128], xT[:, k, c0:c0 + n], k == 0, k == 7)
                    act(sgb[s][:, 0:n], pg, AF.Silu)
                    tt("dve", h_[:, fc, 0:n], sgb[s][:, 0:n], pu, ALU.mult)

            ytog = [0]

            def s2(wi_, idx):
                e_, c0, n = wi_
                Wd = wts[e_][2]
                h_ = hT[idx % 2]
                for j in range(n // 128):
                    i = (c0 + j * 128) // 128
                    yb = 4 + 2 * (ytog[0] % 2)
                    ytog[0] += 1
                    for hf in range(2):
                        for fc in range(4):
                            mm(ps[:, yb + hf, :], h_[:, fc, j * 128:(j + 1) * 128], Wd[:, fc, hf * 512:(hf + 1) * 512],
                               fc == 0, fc == 3)
                    xt2 = xs[:, i, :].rearrange("p (a b) -> p a b", a=2)
                    stt("dve", xt2, ps[:, yb:yb + 2, :], comb[:, i, e_:e_ + 1], xt2, ALU.mult, ALU.add)
                if (c0, n) == mblocks[-1] and e_ + 2 < NE:
                    wts[e_ + 2] = load_expert(e_ + 2)
                if e_ == NE - 1:
                    def ln2_thread(i, q):
                        for _ in layer_norm_tile(i, l2g_bc[:, :], l2b_bc[:, :], q):
                            yield
                        if not last:
                            for _ in build_xT(i, q, None, None, False):
                                yield
                    tl = [(c0 + j * 128) // 128 for j in range(n // 128)]
                    run_threads([ln2_thread(i, q) for q, i in enumerate(tl)])
                    if last:
                        ov = out_d.rearrange("(i p) d -> p i d", p=128)
                        if tl[0] >= 1:
                            out_dmas.append(dma("sp", ov[:, tl[0] - 1:tl[-1], :], xs[:, tl[0]:tl[-1] + 1, :]))

            for idx, w_ in enumerate(work):
                s1(w_, idx)
                if idx > 0:
                    s2(work[idx - 1], idx - 1)
            s2(work[-1], len(work) - 1)
            chk('L%d experts' % l)

            chk('L%d ln2' % l)
        except _Stop:
            ov = out_d.rearrange("(i p) d -> p i d", p=128)
            for (i0, i1) in ((1, 5), (5, 9), (9, 13), (13, 17)):
                out_dmas.append(dma("sp", ov[:, i0 - 1:i1 - 1, :], xs[:, i0:i1, :]))
        tail_deps = list(out_dmas)
        for _eng in Sched.ENGS:
            _real = [o for o in S.ops[_eng] if o.fn is not None]
            if _real:
                tail_deps.append(_real[-1])
            tail_deps.extend(o for o in S.ops[_eng] if o.dma)
        S.add("sp", None, extra_deps=tail_deps)

        with nc.Block() as block:
            S.emit(nc, block, sems)
    return nc


_NC = None


def kernel(**inputs):
    global _NC
    if _NC is None:
        _NC = build_program()
    x = np.ascontiguousarray(np.asarray(inputs["x"], dtype=np.float32))
    shared = {k: np.ascontiguousarray(np.asarray(v, dtype=np.float32)) for k, v in inputs.items() if k != "x"}
    in_maps = []
    for c in range(NCORES):
        b, q = c // 4, c % 4
        xc = np.zeros((TOK, D), np.float32)
        xc[128:] = x[b, q * 2048:(q + 1) * 2048]
        if q > 0:
            xc[:128] = x[b, q * 2048 - 128:q * 2048]
        m = dict(shared)
        m["x"] = xc
        m["hm"] = np.full((128, 1), 1.0 if q > 0 else 0.0, np.float32)
        in_maps.append(m)
    res = run_bass_kernel_spmd(_NC, in_maps, core_ids=list(range(NCORES)))
    out = np.empty((2, 8192, D), np.float32)
    for c in range(NCORES):
        b, q = c // 4, c % 4
        out[b, q * 2048:(q + 1) * 2048] = res.results[c]["out"]
    return out
```
